# Optimizing a Trainium2 kernel written in Bass

```python
import math
import jax
import jax.numpy as jnp
from jax import lax
import numpy as np

D_MODEL = 2048
BATCH = 8
SEQ = 2048
DEPTH = 4

GRID_W = 64
CTX_LEN = 256
N_MIXERS = 3
N_HGRN = len(range(0, DEPTH, N_MIXERS))
N_DIFF = len(range(1, DEPTH, N_MIXERS))
N_MLSTM = len(range(2, DEPTH, N_MIXERS))

HGRN_HEAD_DIM = 128
HGRN_HEADS = D_MODEL // HGRN_HEAD_DIM
HGRN_WIDTH = HGRN_HEADS * HGRN_HEAD_DIM
HGRN_IN_COLS = 5 * HGRN_WIDTH
HGRN_CHUNK = 32

DIFF_HEAD_DIM = 128
DIFF_HEADS = D_MODEL // (2 * DIFF_HEAD_DIM)
DIFF_WIDTH = 2 * DIFF_HEADS * DIFF_HEAD_DIM
DIFF_IN_COLS = 3 * DIFF_WIDTH
Q_BLOCK = 128
ROPE_BASE = 10000.0

MLSTM_HEADS = 8
MLSTM_V_DIM = D_MODEL // MLSTM_HEADS
MLSTM_QK_DIM = MLSTM_V_DIM // 2
MLSTM_K_W = MLSTM_HEADS * MLSTM_QK_DIM
MLSTM_V_W = MLSTM_HEADS * MLSTM_V_DIM
MLSTM_G_W = 4 * MLSTM_HEADS
MLSTM_CUTS = (MLSTM_K_W, MLSTM_K_W + MLSTM_V_W, MLSTM_K_W + MLSTM_V_W + MLSTM_G_W,
              2 * MLSTM_K_W + MLSTM_V_W + MLSTM_G_W)
MLSTM_IN_COLS = 2 * MLSTM_K_W + 2 * MLSTM_V_W + MLSTM_G_W
MLSTM_CHUNK = 64
GATE_SOFTCAP = 15.0
MLSTM_FGATE_BIAS = 3.0

N_EXPERTS = 16
EC_CAPACITY_FACTOR = 2
EXPERT_FF = 1408

NORM_EPS = 1e-6

kernel_name = "hybrid_hgrn2_diffattn_mlstm_ec_moe_dit"

F32 = jnp.float32


def rms_norm(x, g):
    xf = x.astype(F32)
    y = xf * lax.rsqrt(jnp.mean(xf * xf, axis=-1, keepdims=True) + NORM_EPS)
    return (y * g.astype(F32)).astype(x.dtype)


def modulate(h, shift, scale):
    return h * (1.0 + scale) + shift


def flip_t(a, direction):
    return a if (a is None or direction == 0) else jnp.flip(a, axis=2)


def axial_rope_tables(row, col):
    half = DIFF_HEAD_DIM // 2
    inv = 1.0 / (ROPE_BASE ** (jnp.arange(0, half, 2, dtype=F32) / half))
    ar = row.astype(F32)[:, None] * inv
    ac = col.astype(F32)[:, None] * inv
    return jnp.cos(ar), jnp.sin(ar), jnp.cos(ac), jnp.sin(ac)


def _rotate(x, cos, sin):
    x1, x2 = jnp.split(x, 2, axis=-1)
    return jnp.concatenate([x1 * cos - x2 * sin, x2 * cos + x1 * sin], axis=-1)


def apply_axial_rope(x, tables):
    cr, sr, cc, sc = tables
    xr, xcol = jnp.split(x, 2, axis=-1)
    return jnp.concatenate([_rotate(xr, cr, sr), _rotate(xcol, cc, sc)], axis=-1).astype(x.dtype)


def hgrn_lower_bounds(logits):
    p = jax.nn.softmax(logits.astype(F32), axis=0)
    return jnp.cumsum(p, axis=0) - p[0]


def gated_linear_scan(q, k, v, log_f, s0):
    B, H, T, _ = k.shape
    L = HGRN_CHUNK
    n = T // L

    def chunk(a):
        return jnp.moveaxis(a.astype(F32).reshape(B, H, n, L, a.shape[-1]), 2, 0)

    causal = jnp.tril(jnp.ones((L, L), bool))[:, :, None]
    with_out = q is not None
    xs = (chunk(k), chunk(v), chunk(log_f)) + ((chunk(q),) if with_out else ())

    def step(s, inp):
        kc, vc, gc = inp[:3]
        b = jnp.cumsum(gc, axis=2)
        b_last = b[:, :, -1:, :]
        s_new = (jnp.exp(jnp.swapaxes(b_last, 2, 3)) * s
                 + jnp.einsum('bhld,bhle->bhde', kc * jnp.exp(b_last - b), vc))
        if not with_out:
            return s_new, None
        qc = inp[3]
        rel = jnp.exp(jnp.where(causal, b[:, :, :, None, :] - b[:, :, None, :, :], -jnp.inf))
        attn = jnp.einsum('bhtsd,bhsd->bhts', qc[:, :, :, None, :] * rel, kc)
        o = (jnp.einsum('bhtd,bhde->bhte', qc * jnp.exp(b), s)
             + jnp.einsum('bhts,bhse->bhte', attn, vc))
        return s_new, o

    s_final, o = lax.scan(step, s0, xs)
    if with_out:
        o = jnp.moveaxis(o, 0, 2).reshape(B, H, T, -1)
    return s_final, o


def mlstm_scan(q, k, v, log_i, log_f, state0):
    B, H, T, _ = k.shape
    L = MLSTM_CHUNK
    n_chunks = T // L

    def chunk(a):
        return jnp.moveaxis(a.astype(F32).reshape(B, H, n_chunks, L, *a.shape[3:]), 2, 0)

    causal = jnp.tril(jnp.ones((L, L), bool))
    with_out = q is not None
    xs = (chunk(k), chunk(v), chunk(log_i), chunk(log_f)) + ((chunk(q),) if with_out else ())

    def step(state, inp):
        C, nrm, m = state
        kc, vc, ic, fc = inp[:4]
        b = jnp.cumsum(fc, axis=-1)
        b_last = b[..., -1]
        w_end = b_last[..., None] - b + ic
        m_new = jnp.maximum(b_last + m, jnp.max(w_end, axis=-1))
        carry = jnp.exp(b_last + m - m_new)
        k_end = kc * jnp.exp(w_end - m_new[..., None])[..., None]
        C_new = carry[..., None, None] * C + jnp.einsum('bhld,bhle->bhde', k_end, vc)
        n_new = carry[..., None] * nrm + jnp.sum(k_end, axis=2)
        if not with_out:
            return (C_new, n_new, m_new), None
        qc = inp[4]
        logw = jnp.where(causal, b[..., :, None] - b[..., None, :] + ic[..., None, :], -jnp.inf)
        m_inter = b + m[..., None]
        m_row = jnp.maximum(m_inter, jnp.max(logw, axis=-1))
        p = jnp.exp(logw - m_row[..., None]) * jnp.einsum('bhtd,bhsd->bhts', qc, kc)
        a_inter = jnp.exp(m_inter - m_row)
        num = (a_inter[..., None] * jnp.einsum('bhtd,bhde->bhte', qc, C)
               + jnp.einsum('bhts,bhse->bhte', p, vc))
        den = a_inter * jnp.einsum('bhtd,bhd->bht', qc, nrm) + jnp.sum(p, axis=-1)
        h = num / jnp.maximum(jnp.abs(den), jnp.exp(-m_row))[..., None]
        return (C_new, n_new, m_new), h

    state, h = lax.scan(step, state0, xs)
    if with_out:
        h = jnp.moveaxis(h, 0, 2).reshape(B, H, T, -1)
    return state, h


def hgrn2_mixer(h_ctx, h_lat, w_in, lb, norm_g, w_out, full_ctx):
    W = HGRN_WIDTH
    z_lat = h_lat @ w_in
    z_ctx = h_ctx @ (w_in if full_ctx else w_in[:, :3 * W])

    def col(z, i):
        return z[..., i * W:(i + 1) * W]

    def heads(a):
        return jnp.swapaxes(a.reshape(a.shape[0], a.shape[1], HGRN_HEADS, HGRN_HEAD_DIM), 1, 2)

    def forget(z, lb_dir):
        zf = z.astype(F32)
        log_f = jnp.logaddexp(jnp.log(lb_dir), jnp.log1p(-lb_dir) + jax.nn.log_sigmoid(zf))
        key = (1.0 - lb_dir) * jax.nn.sigmoid(-zf)
        return heads(log_f), heads(key)

    v_lat, v_ctx = heads(col(z_lat, 2)), heads(col(z_ctx, 2))
    q_lat = heads(jax.nn.silu(col(z_lat, 3)))
    q_ctx = heads(jax.nn.silu(col(z_ctx, 3))) if full_ctx else None
    B = h_lat.shape[0]
    o_lat, o_ctx = [], []
    for d in range(2):
        lf_c, k_c = forget(col(z_ctx, d), lb[d])
        lf_l, k_l = forget(col(z_lat, d), lb[d])
        s0 = jnp.zeros((B, HGRN_HEADS, HGRN_HEAD_DIM, HGRN_HEAD_DIM), F32)
        s_ctx, oc = gated_linear_scan(flip_t(q_ctx, d), flip_t(k_c, d), flip_t(v_ctx, d), flip_t(lf_c, d), s0)
        _, ol = gated_linear_scan(flip_t(q_lat, d), flip_t(k_l, d), flip_t(v_lat, d), flip_t(lf_l, d), s_ctx)
        o_lat.append(flip_t(ol, d))
        if full_ctx:
            o_ctx.append(flip_t(oc, d))

    def readout(o, z):
        o = rms_norm(jnp.swapaxes(o, 1, 2), norm_g)
        g = jax.nn.silu(col(z, 4).astype(F32)).reshape(o.shape)
        return (o * g).reshape(o.shape[0], o.shape[1], W).astype(z.dtype) @ w_out

    out_ctx = readout(o_ctx[0] + o_ctx[1], z_ctx) if full_ctx else None
    return out_ctx, readout(o_lat[0] + o_lat[1], z_lat)


def diff_attend(q, k, v, lam):
    B, H2, Lq, d = q.shape
    s = jnp.einsum('bhqd,bhkd->bhqk', q, k, preferred_element_type=F32) * (d ** -0.5)
    p = jax.nn.softmax(s, axis=-1).reshape(B, H2 // 2, 2, Lq, -1)
    w = p[:, :, 0] - lam * p[:, :, 1]
    return jnp.einsum('bhqk,bhkd->bhqd', w, v.astype(F32))


def diff_attn_mixer(h_ctx, h_lat, w_in, lam_p, norm_g, w_out, rope, layer_idx, full_ctx):
    W, H, d = DIFF_WIDTH, DIFF_HEADS, DIFF_HEAD_DIM
    lam_init = 0.8 - 0.6 * math.exp(-0.3 * layer_idx)
    lp = lam_p.astype(F32)
    lam = jnp.exp(jnp.sum(lp[0] * lp[1])) - jnp.exp(jnp.sum(lp[2] * lp[3])) + lam_init
    z_lat = h_lat @ w_in
    z_ctx = h_ctx @ (w_in if full_ctx else w_in[:, :2 * W])

    def col(z, i):
        return z[..., i * W:(i + 1) * W]

    def sub(a):
        return jnp.swapaxes(a.reshape(a.shape[0], a.shape[1], 2 * H, d), 1, 2)

    def val(a):
        return jnp.swapaxes(a.reshape(a.shape[0], a.shape[1], H, 2 * d), 1, 2)

    k_ctx, v_ctx = sub(col(z_ctx, 0)), val(col(z_ctx, 1))
    k_lat = apply_axial_rope(sub(col(z_lat, 0)), rope)
    v_lat = val(col(z_lat, 1))
    q_lat = apply_axial_rope(sub(col(z_lat, 2)), rope)
    k_all = jnp.concatenate([k_ctx, k_lat], axis=2)
    v_all = jnp.concatenate([v_ctx, v_lat], axis=2)
    B, _, T, _ = q_lat.shape
    nb = T // Q_BLOCK
    qb = jnp.moveaxis(q_lat.reshape(B, 2 * H, nb, Q_BLOCK, d), 2, 0)
    o_lat = lax.map(lambda qq: diff_attend(qq, k_all, v_all, lam), qb)
    o_lat = jnp.moveaxis(o_lat, 0, 2).reshape(B, H, T, 2 * d)

    def readout(o, dtype):
        o = rms_norm(jnp.swapaxes(o, 1, 2), norm_g) * (1.0 - lam_init)
        return o.reshape(o.shape[0], o.shape[1], W).astype(dtype) @ w_out

    out_ctx = readout(diff_attend(sub(col(z_ctx, 2)), k_ctx, v_ctx, lam), h_ctx.dtype) if full_ctx else None
    return out_ctx, readout(o_lat, h_lat.dtype)


def mlstm_mixer(h_ctx, h_lat, w_in, gate_b, norm_g, w_out, full_ctx):
    H, dk, dv = MLSTM_HEADS, MLSTM_QK_DIM, MLSTM_V_DIM
    c0, c1, c2, c3 = MLSTM_CUTS
    z_lat = h_lat @ w_in
    z_ctx = h_ctx @ (w_in if full_ctx else w_in[:, :c2])

    def heads(a, dh):
        return jnp.swapaxes(a.reshape(a.shape[0], a.shape[1], H, dh), 1, 2)

    def unpack(z):
        k = heads(z[..., :c0], dk)
        v = heads(z[..., c0:c1], dv)
        g = z[..., c1:c2].astype(F32).reshape(z.shape[0], z.shape[1], 4, H) + gate_b.astype(F32)
        g = jnp.transpose(GATE_SOFTCAP * jnp.tanh(g / GATE_SOFTCAP), (2, 0, 3, 1))
        q = heads(z[..., c2:c3], dk) * (dk ** -0.5) if z.shape[-1] > c2 else None
        return q, k, v, g

    q_c, k_c, v_c, g_c = unpack(z_ctx)
    q_l, k_l, v_l, g_l = unpack(z_lat)
    B = h_lat.shape[0]
    h_lat_dirs, h_ctx_dirs = [], []
    for d in range(2):
        state0 = (jnp.zeros((B, H, dk, dv), F32), jnp.zeros((B, H, dk), F32), jnp.zeros((B, H), F32))
        st, hc = mlstm_scan(flip_t(q_c, d), flip_t(k_c, d), flip_t(v_c, d), flip_t(g_c[2 * d], d),
                            flip_t(jax.nn.log_sigmoid(g_c[2 * d + 1]), d), state0)
        _, hl = mlstm_scan(flip_t(q_l, d), flip_t(k_l, d), flip_t(v_l, d), flip_t(g_l[2 * d], d),
                           flip_t(jax.nn.log_sigmoid(g_l[2 * d + 1]), d), st)
        h_lat_dirs.append(flip_t(hl, d))
        if full_ctx:
            h_ctx_dirs.append(flip_t(hc, d))

    def readout(hs, z):
        hh = rms_norm(jnp.swapaxes(hs, 1, 2), norm_g.reshape(H, dv))
        o = jax.nn.sigmoid(z[..., c3:].astype(F32)).reshape(hh.shape)
        return (hh * o).reshape(hh.shape[0], hh.shape[1], H * dv).astype(z.dtype) @ w_out

    out_ctx = readout(h_ctx_dirs[0] + h_ctx_dirs[1], z_ctx) if full_ctx else None
    return out_ctx, readout(h_lat_dirs[0] + h_lat_dirs[1], z_lat)


def ec_moe(h, w_router, w_gate, w_up, w_down):
    B, T, D = h.shape
    cap = EC_CAPACITY_FACTOR * T // N_EXPERTS
    aff = jax.nn.softmax(jnp.einsum('btd,de->bte', h, w_router, preferred_element_type=F32), axis=-1)
    g, idx = lax.top_k(jnp.swapaxes(aff, 1, 2), cap)
    xs = jax.vmap(lambda hb, ib: hb[ib])(h, idx)
    a = jnp.einsum('becd,edf->becf', xs, w_gate)
    u = jnp.einsum('becd,edf->becf', xs, w_up)
    y = jnp.einsum('becf,efd->becd', jax.nn.silu(a) * u, w_down) * g[..., None]
    flat = (jnp.arange(B)[:, None, None] * T + idx).reshape(-1)
    out = jnp.zeros((B * T, D), y.dtype).at[flat].add(y.reshape(-1, D))
    return out.reshape(B, T, D).astype(h.dtype)


def setup_inputs(seed: int = 0) -> dict:
    key = jax.random.key(seed)
    keys = iter(jax.random.split(key, 32))

    def normal(shape, scale):
        return jax.random.normal(next(keys), shape, jnp.float32) * scale

    D = D_MODEL
    fgate_offset = jnp.array([0.0, MLSTM_FGATE_BIAS, 0.0, MLSTM_FGATE_BIAS], jnp.float32)[None, :, None]
    return {
        "x": normal((BATCH, SEQ, D), 1.0),
        "c": normal((BATCH, D), 1.0),
        "ctx": normal((BATCH, CTX_LEN, D), 1.0),
        "c_ctx": normal((D,), 1.0),
        "ada_w": normal((DEPTH, D, 6 * D), 0.5 * D ** -0.5),
        "ada_b": normal((DEPTH, 6 * D), 0.02),
        "norm_g": 1.0 + normal((DEPTH, 2, D), 0.02),
        "final_g": 1.0 + normal((D,), 0.02),
        "hgrn_w_in": normal((N_HGRN, D, HGRN_IN_COLS), D ** -0.5),
        "hgrn_lb_logits": normal((N_HGRN, 2, HGRN_WIDTH), 0.5),
        "hgrn_norm_g": 1.0 + normal((N_HGRN, HGRN_HEAD_DIM), 0.02),
        "hgrn_w_out": normal((N_HGRN, HGRN_WIDTH, D), HGRN_WIDTH ** -0.5),
        "diff_w_in": normal((N_DIFF, D, DIFF_IN_COLS), D ** -0.5),
        "diff_lambda": normal((N_DIFF, 4, DIFF_HEAD_DIM), 0.1),
        "diff_norm_g": 1.0 + normal((N_DIFF, 2 * DIFF_HEAD_DIM), 0.02),
        "diff_w_out": normal((N_DIFF, DIFF_WIDTH, D), DIFF_WIDTH ** -0.5),
        "mlstm_w_in": normal((N_MLSTM, D, MLSTM_IN_COLS), D ** -0.5),
        "mlstm_gate_b": normal((N_MLSTM, 4, MLSTM_HEADS), 0.1) + fgate_offset,
        "mlstm_norm_g": 1.0 + normal((N_MLSTM, MLSTM_V_W), 0.02),
        "mlstm_w_out": normal((N_MLSTM, MLSTM_V_W, D), MLSTM_V_W ** -0.5),
        "moe_router": normal((DEPTH, D, N_EXPERTS), D ** -0.5),
        "moe_w_gate": normal((DEPTH, N_EXPERTS, D, EXPERT_FF), D ** -0.5),
        "moe_w_up": normal((DEPTH, N_EXPERTS, D, EXPERT_FF), D ** -0.5),
        "moe_w_down": normal((DEPTH, N_EXPERTS, EXPERT_FF, D), EXPERT_FF ** -0.5),
    }


def reference(x, c, ctx, c_ctx, ada_w, ada_b, norm_g, final_g,
              hgrn_w_in, hgrn_lb_logits, hgrn_norm_g, hgrn_w_out,
              diff_w_in, diff_lambda, diff_norm_g, diff_w_out,
              mlstm_w_in, mlstm_gate_b, mlstm_norm_g, mlstm_w_out,
              moe_router, moe_w_gate, moe_w_up, moe_w_down):
    B, T, D = x.shape
    rows = T // GRID_W
    row = jnp.repeat(jnp.arange(rows), GRID_W)
    col = jnp.tile(jnp.arange(GRID_W), rows)
    rope = axial_rope_tables(row, col)
    lb_all = hgrn_lower_bounds(hgrn_lb_logits)
    s_c = jax.nn.silu(c)
    s_cc = jax.nn.silu(c_ctx)
    xc = ctx
    for layer in range(DEPTH):
        full_ctx = layer < DEPTH - 1
        mod = s_c @ ada_w[layer] + ada_b[layer]
        sh1, sc1, g1, sh2, sc2, g2 = jnp.split(mod[:, None, :], 6, axis=-1)
        n_mod_c = 6 if full_ctx else 2
        mod_c = s_cc @ ada_w[layer][:, :n_mod_c * D] + ada_b[layer][:n_mod_c * D]
        mc = jnp.split(mod_c, n_mod_c)

        h = modulate(rms_norm(x, norm_g[layer, 0]), sh1, sc1)
        hc = modulate(rms_norm(xc, norm_g[layer, 0]), mc[0], mc[1])
        kind, j = layer % N_MIXERS, layer // N_MIXERS
        if kind == 0:
            oc, o = hgrn2_mixer(hc, h, hgrn_w_in[j], lb_all[j], hgrn_norm_g[j], hgrn_w_out[j], full_ctx)
        elif kind == 1:
            oc, o = diff_attn_mixer(hc, h, diff_w_in[j], diff_lambda[j], diff_norm_g[j], diff_w_out[j],
                                    rope, layer, full_ctx)
        else:
            oc, o = mlstm_mixer(hc, h, mlstm_w_in[j], mlstm_gate_b[j], mlstm_norm_g[j], mlstm_w_out[j], full_ctx)
        x = x + g1 * o
        h = modulate(rms_norm(x, norm_g[layer, 1]), sh2, sc2)
        x = x + g2 * ec_moe(h, moe_router[layer], moe_w_gate[layer], moe_w_up[layer], moe_w_down[layer])
        if full_ctx:
            xc = xc + mc[2] * oc
            hc = modulate(rms_norm(xc, norm_g[layer, 1]), mc[3], mc[4])
            xc = xc + mc[5] * ec_moe(hc, moe_router[layer], moe_w_gate[layer], moe_w_up[layer], moe_w_down[layer])
    return rms_norm(x, final_g)
```

```python
import numpy as np
from contextlib import ExitStack
import concourse.bass as bass
import concourse.mybir as mybir
from concourse.bass_utils import run_bass_kernel_spmd

F32 = mybir.dt.float32
F32R = mybir.dt.float32r
BF16 = mybir.dt.bfloat16
I32 = mybir.dt.int32
U32 = mybir.dt.uint32
U8 = mybir.dt.uint8
AF = mybir.ActivationFunctionType
ALU = mybir.AluOpType
AX = mybir.AxisListType


class FW:
    def __init__(self, nc, es, n_dma_sems=40):
        self.nc = nc
        self.es = es
        self.E = {'pe': nc.tensor, 'dve': nc.vector, 'act': nc.scalar,
                  'pool': nc.gpsimd, 'sp': nc.sync}
        self.esem = {k: es.enter_context(nc.semaphore('s_' + k)) for k in self.E}
        self.ecnt = {k: 0 for k in self.E}
        self.waited = {k: {} for k in self.E}
        self.lastw = {}
        self.readers = {}
        self.dsem = [es.enter_context(nc.semaphore('d%d' % i)) for i in range(n_dma_sems)]
        self.dcnt = [0] * n_dma_sems
        self.drr = 0
        self.nwaits = 0
        self.ninst = 0

    def _deps(self, R, W):
        deps = []
        for k in R:
            t = self.lastw.get(k)
            if t is not None:
                deps.append(t)
        for k in W:
            t = self.lastw.get(k)
            if t is not None:
                deps.append(t)
            rd = self.readers.get(k)
            if rd:
                deps.extend(rd.values())
        return deps

    def _wait(self, eng, deps):
        w = self.waited[eng]
        best = {}
        for (sem, val, deng, sid) in deps:
            if deng == eng and eng == 'pe':
                continue
            if w.get(sid, 0) >= val:
                continue
            if sid not in best or best[sid][1] < val:
                best[sid] = (sem, val)
        for sid, (sem, val) in best.items():
            self.E[eng].wait_ge(sem, val)
            w[sid] = val
            self.nwaits += 1

    def _commit(self, tok, R, W):
        for k in W:
            self.lastw[k] = tok
            self.readers[k] = {}
        for k in R:
            self.readers.setdefault(k, {})[tok[3]] = tok

    def op(self, eng, fn, R=(), W=()):
        self._wait(eng, self._deps(R, W))
        inst = fn(self.E[eng])
        self.ecnt[eng] += 1
        inst.then_inc(self.esem[eng], 1)
        self.ninst += 1
        tok = (self.esem[eng], self.ecnt[eng], eng, 'e_' + eng)
        self._commit(tok, R, W)
        return inst

    def dma(self, q, out, in_, R=(), W=(), fn=None, **kw):
        deps = self._deps(R, W)
        i = self.drr
        self.drr = (self.drr + 1) % len(self.dsem)
        if self.dcnt[i] > 0:
            deps.append((self.dsem[i], 16 * self.dcnt[i], 'dma', 'd%d' % i))
        self._wait(q, deps)
        if fn is None:
            inst = self.E[q].dma_start(out=out, in_=in_, **kw)
        else:
            inst = fn(self.E[q])
        self.dcnt[i] += 1
        inst.then_inc(self.dsem[i], 16)
        self.ninst += 1
        tok = (self.dsem[i], 16 * self.dcnt[i], 'dma', 'd%d' % i)
        self._commit(tok, R, W)
        return inst

    def barrier(self):
        deps = []
        for k in self.E:
            if self.ecnt[k] > 0:
                deps.append((self.esem[k], self.ecnt[k], 'x', 'e_' + k))
        for i, s in enumerate(self.dsem):
            if self.dcnt[i] > 0:
                deps.append((s, 16 * self.dcnt[i], 'dma', 'd%d' % i))
        for k in self.E:
            self._wait(k, deps)
        self.lastw.clear()
        self.readers.clear()

    def final_wait(self, eng='sp'):
        deps = []
        for k in self.E:
            if self.ecnt[k] > 0:
                deps.append((self.esem[k], self.ecnt[k], 'x', 'e_' + k))
        for i, s in enumerate(self.dsem):
            if self.dcnt[i] > 0:
                deps.append((s, 16 * self.dcnt[i], 'dma', 'd%d' % i))
        self._wait(eng, deps)


T_CTX = 256
T_LAT = 2048
TT = 2304
NT = 18
D = 2048
KC = 16
EPS = 1e-6
DEPTH = 4
NE = 16
FF = 1408
NF = 11


class Builder:
    def __init__(self, nlayers=4, dbg=False, stop_after=None):
        self.nlayers = nlayers
        self.dbg = dbg
        self.stop_after = stop_after
        nc = bass.Bass("TRN2", target_bir_lowering=False)
        self.nc = nc
        self.shapes = {}
        def inp(name, shape, dt=F32):
            self.shapes[name] = (list(shape), dt)
        inp("x", [T_LAT, D]); inp("ctx", [T_CTX, D]); inp("cc", [2, D])
        inp("ada_w", [DEPTH, D, 6 * D]); inp("ada_b", [DEPTH, 6 * D]); inp("norm_g", [8, D]); inp("final_g", [1, D])
        inp("hgrn_w_in", [2, D, 10240]); inp("hgrn_lb", [4, D]); inp("hgrn_ng", [2, 128]); inp("hgrn_w_out", [2, D, D])
        inp("diff_w_in", [1, D, 6144]); inp("diff_lam", [4, 128]); inp("diff_ng", [1, 256]); inp("diff_w_out", [1, D, D])
        inp("mlstm_w_in", [1, D, 6176]); inp("mlstm_gb", [32, 1]); inp("mlstm_ng", [1, D]); inp("mlstm_w_out", [1, D, D])
        inp("moe_router", [DEPTH, D, NE]); inp("moe_w_gate", [DEPTH, NE, D, FF]); inp("moe_w_up", [DEPTH, NE, D, FF])
        inp("moe_w_down", [DEPTH, NE, FF, D])
        inp("ident", [128, 128]); inp("mask_f", [128, 128], U8); inp("mask_b", [128, 128], U8)
        inp("rope_c", [128, T_LAT]); inp("rope_s", [128, T_LAT]); inp("perm", [128, 128]); inp("gsel", [32, 32 * 128])
        outer = self
        class LazyIn(dict):
            def __missing__(d, name):
                shape, dt = outer.shapes[name]
                ap = nc.dram_tensor(name, shape, dt, kind="ExternalInput").ap()
                d[name] = ap
                return ap
        self.I = LazyIn()
        self.out = nc.dram_tensor("out", [T_LAT, D], F32, kind="ExternalOutput").ap()
        if dbg:
            self.dbg_x = nc.dram_tensor("dbg_x", [TT, D], F32, kind="ExternalOutput").ap()
        self.XA = nc.dram_tensor("XA", [TT, D], F32).ap()
        self.XB = nc.dram_tensor("XB", [TT, D], F32).ap()
        self.HT = nc.dram_tensor("HT", [KC, 128, TT], BF16).ap()
        self.OGT = nc.dram_tensor("OGT", [KC, 128, TT], BF16).ap()
        self.MOD = nc.dram_tensor("MOD", [2, 6 * D], F32).ap()

    def sb(self, es, name, shape, dt):
        self.nsb = getattr(self, 'nsb', 0) + 1
        return es.enter_context(self.nc.sbuf_tensor("sb%d_%s" % (self.nsb, name), list(shape), dt))

    def src_rows(self, layer, i):
        if layer == 0:
            if i < 2:
                return self.I["ctx"][i * 128:(i + 1) * 128, :], 'in_ctx'
            return self.I["x"][(i - 2) * 128:(i - 1) * 128, :], 'in_x'
        return self.XB[i * 128:(i + 1) * 128, :], 'XB%d' % i

    def rows2cols(self, src, R, n, dst, bank, keyR, keyW):
        fw = self.fw
        pk = bank[1]
        pt = bank[0]
        for j in range(n):
            fw.op('pe', lambda e, j=j: e.transpose(pt[:, j * R:(j + 1) * R], src[0:R, j * 128:(j + 1) * 128],
                                                   self.ident[0:R, 0:R]), R=[keyR, 'ident'], W=[pk])
        fw.op('dve', lambda e: e.tensor_copy(dst[:].rearrange("p n r -> p (n r)"), pt[:, 0:n * R]), R=[pk], W=[keyW])

    def build(self):
        nc = self.nc
        with ExitStack() as es:
            self.fw = fw = FW(nc, es)
            self.ident = self.sb(es, "ident", [128, 128], F32)
            self.identb = self.sb(es, "identb", [128, 128], BF16)
            self.maskf = self.sb(es, "maskf", [128, 128], U8)
            self.maskb = self.sb(es, "maskb", [128, 128], U8)
            self.zerob = self.sb(es, "zerob", [128, 128], BF16)
            self.S2 = self.sb(es, "S2", [128, KC, 2], BF16)
            self.ngcol = self.sb(es, "ngcol", [128, KC, 8], F32)
            self.modcol = self.sb(es, "modcol", [128, 96, 2], F32)
            self.AB = self.sb(es, "AB", [128, 8, KC], F32)
            self.epsc = self.sb(es, "epsc", [128, 1], F32)
            self.onec = self.sb(es, "onec", [128, 1], F32)
            self.lbcol = self.sb(es, "lbcol", [128, KC, 4], F32)
            self.lb1 = self.sb(es, "lb1", [128, KC, 2], F32)
            self.oml1 = self.sb(es, "oml1", [128, KC, 2], F32)
            self.P0 = es.enter_context(nc.psum_tensor("P0", [128, 2048], F32))
            self.P1 = es.enter_context(nc.psum_tensor("P1", [128, 2048], F32))
            self.banks = [(self.P0[:, j * 512:(j + 1) * 512], 'pb%d' % j) for j in range(4)] + \
                         [(self.P1[:, j * 512:(j + 1) * 512], 'pb%d' % (4 + j)) for j in range(4)]
            fw.dma('sp', self.ident[:], self.I["ident"], W=['ident'])
            fw.dma('sp', self.maskf[:], self.I["mask_f"], W=['maskf'])
            fw.dma('sp', self.maskb[:], self.I["mask_b"], W=['maskb'])
            fw.op('dve', lambda e: e.tensor_copy(self.identb[:], self.ident[:]), R=['ident'], W=['identb'])
            fw.op('dve', lambda e: e.memset(self.zerob[:], 0.0), W=['zerob'])
            fw.op('dve', lambda e: e.memset(self.epsc[:], EPS), W=['eps'])
            fw.op('dve', lambda e: e.memset(self.onec[:], 1.0), W=['onec'])
            self.prologue()
            for layer in range(self.nlayers):
                self.adaln(layer)
                self.phaseA(layer)
                kind = layer % 3
                if kind == 0:
                    self.hgrn(layer)
                elif kind == 1:
                    self.diffattn(layer)
                else:
                    self.mlstm(layer)
                self.phaseC(layer)
                if self.dbg:
                    o = nc.dram_tensor("dbg_xm%d" % layer, [TT, D], F32, kind="ExternalOutput").ap()
                    for i in range(NT):
                        fw.dma('sp', o[i * 128:(i + 1) * 128, :], self.XB[i * 128:(i + 1) * 128, :])
                    fw.barrier()
                if self.stop_after == ('mix', layer):
                    break
                self.moe(layer)
                if self.dbg:
                    o = nc.dram_tensor("dbg_x%d" % layer, [TT, D], F32, kind="ExternalOutput").ap()
                    for i in range(NT):
                        fw.dma('sp', o[i * 128:(i + 1) * 128, :], self.XB[i * 128:(i + 1) * 128, :])
                    fw.barrier()
            if self.dbg:
                fw.barrier()
                for nm, t in (("MOD", self.MOD), ("HT", self.HT), ("OGT", self.OGT), ("XA", self.XA)):
                    o = nc.dram_tensor("dbg_" + nm, list(t.shape), t.dtype, kind="ExternalOutput").ap()
                    fw.dma('sp', o, t)
                for i in range(NT):
                    fw.dma('sp', self.dbg_x[i * 128:(i + 1) * 128, :], self.XB[i * 128:(i + 1) * 128, :], R=['XB'])
            else:
                self.final_norm()
            fw.final_wait('sp')
        return nc

    def prologue(self):
        fw = self.fw
        with ExitStack() as es:
            rows = self.sb(es, "pr_rows", [8, D], F32)
            cols = self.sb(es, "pr_cols", [128, KC, 8], F32)
            fw.dma('sp', rows[:], self.I["norm_g"], W=['pr_rows'])
            self.rows2cols(rows, 8, KC, self.ngcol, self.banks[0], 'pr_rows', 'ngcol')
            fw.dma('sp', rows[0:2, :], self.I["cc"], W=['pr_rows'])
            c2 = self.sb(es, "pr_c2", [128, KC, 2], F32)
            self.rows2cols(rows, 2, KC, c2, self.banks[1], 'pr_rows', 'pr_c2')
            fw.op('act', lambda e: e.activation(self.S2[:], c2[:], AF.Silu), R=['pr_c2'], W=['S2'])
            fw.dma('sp', rows[0:4, :], self.I["hgrn_lb"], W=['pr_rows'])
            self.rows2cols(rows, 4, KC, self.lbcol, self.banks[2], 'pr_rows', 'lbcol')
            ex = self.sb(es, "pr_ex", [128, KC, 4], F32)
            fw.op('act', lambda e: e.activation(ex[:], self.lbcol[:], AF.Exp), R=['lbcol'], W=['pr_ex'])
            sm = self.sb(es, "pr_sm", [128, KC, 2], F32)
            fw.op('dve', lambda e: e.tensor_tensor(sm[:], ex[:, :, 0:2], ex[:, :, 2:4], ALU.add), R=['pr_ex'], W=['pr_sm'])
            fw.op('dve', lambda e: e.reciprocal(sm[:], sm[:]), R=['pr_sm'], W=['pr_sm'])
            fw.op('dve', lambda e: e.tensor_tensor(self.lb1[:], ex[:, :, 2:4], sm[:], ALU.mult), R=['pr_ex', 'pr_sm'], W=['lb1'])
            fw.op('dve', lambda e: e.tensor_scalar(self.oml1[:], self.lb1[:], -1.0, 1.0, ALU.mult, ALU.add), R=['lb1'], W=['oml1'])
            fw.barrier()

    def adaln(self, layer):
        fw = self.fw
        nc = self.nc
        with ExitStack() as es:
            wb = [self.sb(es, "ad_w%d" % i, [128, KC, 512], BF16) for i in range(2)]
            modrow = self.sb(es, "ad_mod", [2, 6 * D], F32)
            bias = self.sb(es, "ad_b", [2, 6 * D], F32)
            fw.dma('sp', bias[:], self.I["ada_b"][layer:layer + 1, :].partition_broadcast(2)[:, 0, :], W=['ad_b'])
            for nb in range(24):
                b = nb % 2
                src = self.I["ada_w"][layer, :, nb * 512:(nb + 1) * 512].rearrange("(k p) f -> p k f", p=128)
                fw.dma('pool', wb[b][:], src, W=['ad_w%d' % b])
                bk = self.banks[nb % 4]
                for k in range(KC):
                    fw.op('pe', lambda e, k=k: e.matmul(bk[0][0:2, :], lhsT=self.S2[:, k, :], rhs=wb[b][:, k, :],
                                                        start=(k == 0), stop=(k == KC - 1)),
                          R=['S2', 'ad_w%d' % b], W=[bk[1]])
                fw.op('dve', lambda e: e.tensor_tensor(modrow[:, nb * 512:(nb + 1) * 512], bk[0][0:2, :],
                                                       bias[:, nb * 512:(nb + 1) * 512], ALU.add),
                      R=[bk[1], 'ad_b'], W=['ad_mod'])
            fw.dma('sp', self.MOD, modrow[:], R=['ad_mod'], W=['MOD'])
            self.rows2cols(modrow, 2, 96, self.modcol, self.banks[4], 'ad_mod', 'modcol')
            mc = self.modcol
            AB = self.AB

            def seg(s, r):
                return mc[:, 16 * s:16 * s + 16, r]
            for which in range(2):
                g = self.ngcol[:, :, layer * 2 + which]
                for r in range(2):
                    ia = which * 4 + r * 2
                    sh, sc = seg(3 * which, r), seg(3 * which + 1, r)
                    fw.op('dve', lambda e, ia=ia, sc=sc, g=g: e.scalar_tensor_tensor(AB[:, ia, :], sc, 1.0, g, ALU.add, ALU.mult),
                          R=['modcol', 'ngcol'], W=['AB'])
                    fw.op('dve', lambda e, ia=ia, sh=sh: e.tensor_copy(AB[:, ia + 1, :], sh), R=['modcol'], W=['AB'])
            fw.barrier()

    def norm_tile(self, xt, kx, nrows, ia, dst, kdst, tmp, ktmp, bank_ids=(0, 1, 2, 3), dst_f32=None):
        fw = self.fw
        ss, sd, junk = tmp
        n = nrows
        fw.op('act', lambda e: e.activation(junk[0:n, :], xt[0:n, :], AF.Square, accum_out=ss[0:n, 0:1]), R=[kx], W=[ktmp])
        fw.op('act', lambda e: e.activation(sd[0:n, :], ss[0:n, :], AF.Sqrt, bias=self.epsc[0:n, :], scale=1.0 / D), R=[ktmp, 'eps'], W=[ktmp + 'd'])
        fw.op('dve', lambda e: e.reciprocal(sd[0:n, :], sd[0:n, :]), R=[ktmp + 'd'], W=[ktmp + 'd'])
        fw.op('dve', lambda e: e.tensor_scalar(xt[0:n, :], xt[0:n, :], sd[0:n, 0:1], None, ALU.mult), R=[kx, ktmp + 'd'], W=[kx])
        for q in range(4):
            bk = self.banks[bank_ids[q]]
            for j in range(4):
                k = q * 4 + j
                fw.op('pe', lambda e, k=k, j=j: e.transpose(bk[0][:, j * 128:j * 128 + n], xt[0:n, k * 128:(k + 1) * 128],
                                                            self.ident[0:n, 0:n]), R=[kx, 'ident'], W=[bk[1]])
            for j in range(4):
                k = q * 4 + j
                fw.op('act', lambda e, k=k, j=j: e.activation(dst[:, k, 0:n], bk[0][:, j * 128:j * 128 + n], AF.Identity,
                                                              bias=self.AB[:, ia + 1, k:k + 1], scale=self.AB[:, ia, k:k + 1]),
                      R=[bk[1], 'AB'], W=[kdst])

    def phaseA(self, layer):
        fw = self.fw
        with ExitStack() as es:
            xt = [self.sb(es, "pa_x%d" % i, [128, D], F32) for i in range(2)]
            ht = [self.sb(es, "pa_h%d" % i, [128, KC, 128], BF16) for i in range(2)]
            ss = self.sb(es, "pa_ss", [128, 1], F32); sd = self.sb(es, "pa_sd", [128, 1], F32)
            junk = self.sb(es, "pa_junk", [128, D], BF16)
            for i in range(NT):
                b = i % 2
                src, ksrc = self.src_rows(layer, i)
                fw.dma('sp', xt[b][:], src, R=[ksrc], W=['pa_x%d' % b])
                self.norm_tile(xt[b], 'pa_x%d' % b, 128, 0 if i >= 2 else 2, ht[b], 'pa_h%d' % b, (ss, sd, junk), 'pa_t',
                               bank_ids=(0, 1, 2, 3) if b == 0 else (4, 5, 6, 7))
                fw.dma('sp', self.HT.rearrange("k p t -> p k t")[:, :, i * 128:(i + 1) * 128], ht[b][:], R=['pa_h%d' % b], W=['HT'])
            fw.barrier()

    def gla(self, es, pfx, E, qt, kt, kh, vt, emid, eend, keys, direction, post):
        fw = self.fw
        if isinstance(es, tuple):
            S, Sb, at = es
        else:
            self.uid = getattr(self, 'uid', 0) + 1
            pfx = pfx + '_%d' % self.uid
            S = self.sb(es, pfx + "_S", [128, E], F32)
            Sb = self.sb(es, pfx + "_Sb", [128, E], BF16)
            at = [self.sb(es, pfx + "_at%d" % i, [128, 128], BF16) for i in range(2)]
        kS, kSb = pfx + '_S', pfx + '_Sb'
        fw.op('dve', lambda e: e.memset(S[:], 0.0), W=[kS])
        for b_ in range(2):
            fw.op('dve', lambda e: e.memset(at[b_][:], 0.0), W=[pfx + '_at%d' % b_])
        mask, kmask = (self.maskf, 'maskf') if direction == 0 else (self.maskb, 'maskb')
        if direction == 0:
            order = list(range(NT))
        else:
            order = [1, 0] + list(range(NT - 1, 1, -1))
        for n, i in enumerate(order):
            b = n % 2
            cs = slice(i * 128, (i + 1) * 128)
            pa, po, pd = self.banks[2 + b], self.banks[4 + b], self.banks[6 + b]
            fw.op('act', lambda e: e.activation(Sb[:], S[:], AF.Identity, scale=emid[:, i:i + 1]), R=[kS, keys['e']], W=[kSb])
            fw.op('pe', lambda e: e.matmul(pa[0][:, 0:128], lhsT=kt[:, cs], rhs=qt[:, cs], start=True, stop=True),
                  R=[keys['k'], keys['q']], W=[pa[1]])
            fw.op('dve', lambda e: e.copy_predicated(at[b][:], mask[:], pa[0][:, 0:128]),
                  R=[pa[1], kmask], W=[pfx + '_at%d' % b])
            fw.op('pe', lambda e: e.matmul(po[0][:, 0:E], lhsT=qt[:, cs], rhs=Sb[:], start=True, stop=False),
                  R=[keys['q'], kSb], W=[po[1]])
            fw.op('pe', lambda e: e.matmul(po[0][:, 0:E], lhsT=at[b][:], rhs=vt[:, i, :], start=False, stop=True),
                  R=[pfx + '_at%d' % b, keys['v']], W=[po[1]])
            post(i, po[0][:, 0:E], po[1])
            fw.op('pe', lambda e: e.matmul(pd[0][:, 0:E], lhsT=kh[:, i, :], rhs=vt[:, i, :], start=True, stop=True),
                  R=[keys['kh'], keys['v']], W=[pd[1]])
            fw.op('dve', lambda e: e.scalar_tensor_tensor(S[:], S[:], eend[:, i:i + 1], pd[0][:, 0:E], ALU.mult, ALU.add),
                  R=[kS, keys['e'], pd[1]], W=[kS])

    def decay_prep(self, pfx, direction, lf, a, tmp, q, k, qt, kt, khT, emid, eend, kin, iw=None):
        fw = self.fw
        a3 = a[:].rearrange("p (n l) -> p n l", l=128)
        t3 = tmp[:].rearrange("p (n l) -> p n l", l=128)
        if direction == 0:
            fw.op('dve', lambda e: e.tensor_tensor_scan(a[:], self.rmask[:], lf[:], 0.0, ALU.mult, ALU.add), R=[kin['lf'], 'rmask'], W=[pfx + 'a'])
            endpos = 127
        else:
            fw.op('dve', lambda e: e.tensor_tensor_scan(a[:, ::-1], self.rmask[:], lf[:, ::-1], 0.0, ALU.mult, ALU.add), R=[kin['lf'], 'rmask'], W=[pfx + 'a'])
            endpos = 0
        mid = a3[:, :, 64:65]
        end = a3[:, :, endpos:endpos + 1]
        fw.op('act', lambda e: e.activation(emid[:].rearrange("p (n o) -> p n o", o=1), mid, AF.Exp), R=[pfx + 'a'], W=[pfx + 'e'])
        fw.op('act', lambda e: e.activation(eend[:].rearrange("p (n o) -> p n o", o=1), end, AF.Exp), R=[pfx + 'a'], W=[pfx + 'e'])
        fw.op('dve', lambda e: e.tensor_tensor(t3, a3, mid.to_broadcast([128, NT, 128]), ALU.subtract), R=[pfx + 'a'], W=[pfx + 't'])
        fw.op('dve', lambda e: e.tensor_scalar(tmp[:], tmp[:], 80.0, -80.0, ALU.min, ALU.max), R=[pfx + 't'], W=[pfx + 't'])
        e1 = self.dp_e1
        fw.op('act', lambda e: e.activation(e1[:], tmp[:], AF.Exp), R=[pfx + 't'], W=['dp_e1'])
        fw.op('dve', lambda e: e.tensor_tensor(qt[:], q[:], e1[:], ALU.mult), R=[kin['q'], 'dp_e1'], W=[pfx + 'qt'])
        if iw is not None:
            fw.op('dve', lambda e: e.tensor_tensor(tmp[:], tmp[:], iw[:], ALU.subtract), R=[pfx + 't', kin['iw']], W=[pfx + 't'])
        fw.op('act', lambda e: e.activation(e1[:], tmp[:], AF.Exp, scale=-1.0), R=[pfx + 't'], W=['dp_e1'])
        fw.op('dve', lambda e: e.tensor_tensor(kt[:], k[:], e1[:], ALU.mult), R=[kin['k'], 'dp_e1'], W=[pfx + 'kt'])
        fw.op('dve', lambda e: e.tensor_tensor(t3, a3, end.to_broadcast([128, NT, 128]), ALU.subtract), R=[pfx + 'a'], W=[pfx + 't'])
        if iw is not None:
            fw.op('dve', lambda e: e.tensor_tensor(tmp[:], tmp[:], iw[:], ALU.subtract), R=[pfx + 't', kin['iw']], W=[pfx + 't'])
        fw.op('act', lambda e: e.activation(e1[:], tmp[:], AF.Exp, scale=-1.0), R=[pfx + 't'], W=['dp_e1'])
        fw.op('dve', lambda e: e.tensor_tensor(khT[:], k[:], e1[:], ALU.mult), R=[kin['k'], 'dp_e1'], W=[pfx + 'khT'])

    def to_tokmajor(self, srcT, ksrc, dst, kdst, bank):
        fw = self.fw
        for g in range(0, NT, 4):
            n = min(4, NT - g)
            pt = bank[0].bitcast(BF16)
            for j in range(n):
                i = g + j
                fw.op('pe', lambda e, i=i, j=j: e.transpose(pt[:, j * 128:(j + 1) * 128], srcT[:, i * 128:(i + 1) * 128], self.identb[:]),
                      R=[ksrc, 'identb'], W=[bank[1]])
            fw.op('act', lambda e, g=g, n=n: e.copy(dst[:, g:g + n, :].rearrange("p n c -> p (n c)"), pt[:, 0:n * 128]), R=[bank[1]], W=[kdst])

    def hgrn(self, layer):
        fw = self.fw
        j = layer // 3
        W_in = self.I["hgrn_w_in"][j]
        with ExitStack() as es:
            wp = self.sb(es, "hg_w", [128, KC, 5, 128], BF16)
            hb = [self.sb(es, "hg_hb%d" % i, [128, KC, 512], BF16) for i in range(2)]
            EF = [[self.sb(es, "hg_ef%d_%d" % (p, d), [128, TT], F32) for d in range(2)] for p in range(2)]
            Q = [self.sb(es, "hg_q%d" % p, [128, TT], BF16) for p in range(2)]
            vt = [self.sb(es, "hg_v%d" % p, [128, NT, 128], BF16) for p in range(2)]
            gs = [self.sb(es, "hg_g%d" % p, [128, NT, 128], BF16) for p in range(2)]
            Kf = self.sb(es, "hg_k", [128, TT], BF16)
            A = self.sb(es, "hg_a", [128, TT], F32)
            TMP = self.sb(es, "hg_tmp", [128, TT], F32)
            self.dp_e1 = self.sb(es, "dp_e1", [128, TT], F32)
            self.rmask = self.sb(es, "rmask", [128, TT], BF16)
            qt = self.sb(es, "hg_qt", [128, TT], BF16); kt = self.sb(es, "hg_kt", [128, TT], BF16)
            khT = self.sb(es, "hg_khT", [128, TT], BF16); kh = self.sb(es, "hg_kh", [128, NT, 128], BF16)
            oacc = self.sb(es, "hg_o", [128, NT, 128], F32)
            emid = self.sb(es, "hg_emid", [128, NT], F32); eend = self.sb(es, "hg_eend", [128, NT], F32)
            ngb = self.sb(es, "hg_ngb", [128, 128], F32)
            ss = self.sb(es, "hg_ss", [128, NT], F32); junk = self.sb(es, "hg_junk", [128, 128], F32)
            og = [self.sb(es, "hg_og%d" % i, [128, 128], BF16) for i in range(2)]
            ogT = self.sb(es, "hg_ogT", [128, TT], BF16)
            gS = self.sb(es, "hg_S", [128, 128], F32); gSb = self.sb(es, "hg_Sb", [128, 128], BF16)
            gat = [self.sb(es, "hg_at%d" % i, [128, 128], BF16) for i in range(2)]
            fw.op('pool', lambda e: e.memset(self.rmask[:], 1.0), W=['rmask'])
            fw.op('pool', lambda e: e.memset(self.rmask[:].rearrange("p (n l) -> p n l", l=128)[:, :, 0:1], 0.0), W=['rmask'])
            fw.dma('sp', ngb[:], self.I["hgrn_ng"][j:j + 1, :].partition_broadcast(128)[:, 0, :], W=['hg_ngb'])
            blocks = [(0, 512), (512, 512), (1024, 512), (1536, 512), (2048, 256)]
            nhb = [0]

            def load_w(hh):
                for g in range(5):
                    src = W_in[:, g * 2048 + hh * 128: g * 2048 + (hh + 1) * 128].rearrange("(k p) c -> p k c", p=128)
                    fw.dma('pool', wp[:, :, g, :], src, W=['hg_w'])

            def inproj(hh, bis):
                p = hh % 2
                w = wp; kw = 'hg_w'
                for bi in bis:
                    t0, bw = blocks[bi]
                    hbi = nhb[0] % 2; nhb[0] += 1
                    h = hb[hbi]; kh_ = 'hg_hb%d' % hbi
                    fw.dma('sp', h[:, :, 0:bw], self.HT.rearrange("k p t -> p k t")[:, :, t0:t0 + bw], R=['HT'], W=[kh_])
                    for gi, g in enumerate((0, 1, 3)):
                        bk = self.banks[gi % 2]
                        for k in range(KC):
                            fw.op('pe', lambda e, k=k, g=g: e.matmul(bk[0][:, 0:bw], lhsT=w[:, k, g, :], rhs=h[:, k, 0:bw],
                                                                     start=(k == 0), stop=(k == KC - 1)), R=[kw, kh_], W=[bk[1]])
                        if g == 3:
                            fw.op('act', lambda e: e.activation(Q[p][:, t0:t0 + bw], bk[0][:, 0:bw], AF.Silu), R=[bk[1]], W=['hg_q%d' % p])
                        else:
                            fw.op('act', lambda e, g=g: e.activation(EF[p][g][:, t0:t0 + bw], bk[0][:, 0:bw], AF.Exp, scale=-1.0),
                                  R=[bk[1]], W=['hg_ef%d_%d' % (p, g)])
                    for tl in range(bw // 128):
                        i = t0 // 128 + tl
                        for gi, g in enumerate((2, 4)):
                            bk = self.banks[2 + gi]
                            for k in range(KC):
                                fw.op('pe', lambda e, k=k, g=g: e.matmul(bk[0][:, 0:128], lhsT=h[:, k, tl * 128:(tl + 1) * 128], rhs=w[:, k, g, :],
                                                                         start=(k == 0), stop=(k == KC - 1)), R=[kw, kh_], W=[bk[1]])
                            if g == 2:
                                fw.op('dve', lambda e: e.tensor_copy(vt[p][:, i, :], bk[0][:, 0:128]), R=[bk[1]], W=['hg_v%d' % p])
                            else:
                                fw.op('act', lambda e: e.activation(gs[p][:, i, :], bk[0][:, 0:128], AF.Silu), R=[bk[1]], W=['hg_g%d' % p])
                if bis[-1] == 4:
                    fw.op('pool', lambda e: e.tensor_tensor(gs[p][:], gs[p][:], ngb[:].rearrange("p (o c) -> p o c", o=1).to_broadcast([128, NT, 128]), ALU.mult),
                          R=['hg_g%d' % p, 'hg_ngb'], W=['hg_g%d' % p])
                    if hh + 1 < 16:
                        load_w(hh + 1)

            load_w(0)
            inproj(0, [0, 1, 2, 3, 4])
            for hh in range(16):
                p = hh % 2
                for d in range(2):
                    ef = EF[p][d]; kef = 'hg_ef%d_%d' % (p, d)
                    fw.op('dve', lambda e: e.tensor_scalar(ef[:], ef[:], 1.0, None, ALU.add), R=[kef], W=[kef])
                    fw.op('dve', lambda e: e.reciprocal(ef[:], ef[:]), R=[kef], W=[kef])
                    if j == 1:
                        fw.op('dve', lambda e: e.tensor_scalar(ef[:], ef[:], self.oml1[:, hh, d:d + 1], self.lb1[:, hh, d:d + 1], ALU.mult, ALU.add),
                              R=[kef, 'lb1', 'oml1'], W=[kef])
                    fw.op('dve', lambda e: e.tensor_scalar(Kf[:], ef[:], -1.0, 1.0, ALU.mult, ALU.add), R=[kef], W=['hg_k'])
                    fw.op('act', lambda e: e.activation(ef[:], ef[:], AF.Ln), R=[kef], W=[kef])
                    pfx = 'hg'
                    self.decay_prep(pfx, d, ef, A, TMP, Q[p], Kf, qt, kt, khT, emid, eend, {'lf': kef, 'q': 'hg_q%d' % p, 'k': 'hg_k'})
                    if hh + 1 < 16:
                        inproj(hh + 1, [0, 1] if d == 0 else [2, 3, 4])
                    self.to_tokmajor(khT, pfx + 'khT', kh, pfx + 'kh', self.banks[0])

                    def post(i, ops, kops, d=d):
                        if d == 0:
                            fw.op('act', lambda e: e.copy(oacc[:, i, :], ops), R=[kops], W=['hg_o'])
                        else:
                            fw.op('dve', lambda e: e.tensor_tensor(oacc[:, i, :], oacc[:, i, :], ops, ALU.add), R=[kops, 'hg_o'], W=['hg_o'])
                    self.gla((gS, gSb, gat), pfx + 'g', 128, qt, kt, kh, vt[p], emid, eend,
                             {'q': pfx + 'qt', 'k': pfx + 'kt', 'kh': pfx + 'kh', 'v': 'hg_v%d' % p, 'e': pfx + 'e'}, d, post)
                for i in range(NT):
                    fw.op('act', lambda e, i=i: e.activation(junk[:], oacc[:, i, :], AF.Square, accum_out=ss[:, i:i + 1]), R=['hg_o'], W=['hg_ss', 'hg_junk'])
                fw.op('act', lambda e: e.activation(ss[:], ss[:], AF.Sqrt, bias=self.epsc[:], scale=1.0 / 128), R=['hg_ss', 'eps'], W=['hg_ss'])
                fw.op('dve', lambda e: e.reciprocal(ss[:], ss[:]), R=['hg_ss'], W=['hg_ss'])
                for i in range(NT):
                    b = i % 2
                    fw.op('dve', lambda e, i=i, b=b: e.scalar_tensor_tensor(og[b][:], oacc[:, i, :], ss[:, i:i + 1], gs[p][:, i, :], ALU.mult, ALU.mult),
                          R=['hg_o', 'hg_ss', 'hg_g%d' % p], W=['hg_og%d' % b])
                    bk = self.banks[b]
                    pt = bk[0].bitcast(BF16)
                    fw.op('pe', lambda e, b=b: e.transpose(pt[:, 0:128], og[b][:], self.identb[:]), R=['hg_og%d' % b, 'identb'], W=[bk[1]])
                    fw.op('act', lambda e, i=i: e.copy(ogT[:, i * 128:(i + 1) * 128], pt[:, 0:128]), R=[bk[1]], W=['hg_ogT'])
                fw.dma('sp', self.OGT[hh], ogT[:], R=['hg_ogT'], W=['OGT'])
            fw.barrier()

    def diffattn(self, layer):
        import math
        fw = self.fw
        j = layer // 3
        W_in = self.I["diff_w_in"][j]
        lam_init = 0.8 - 0.6 * math.exp(-0.3 * layer)
        scale = 128 ** -0.5
        with ExitStack() as es:
            wp = self.sb(es, "da_w", [128, KC, 768], BF16)
            hb = self.sb(es, "da_hb", [128, KC, 512], BF16)
            zT = [self.sb(es, "da_z%d" % i, [128, TT], F32) for i in range(4)]
            fT = [self.sb(es, "da_f%d" % i, [128, TT], BF16) for i in range(4)]
            RC = self.sb(es, "da_rc", [128, T_LAT], F32); RS = self.sb(es, "da_rs", [128, T_LAT], F32)
            permt = self.sb(es, "da_perm", [128, 128], F32)
            t1 = self.sb(es, "da_t1", [128, 512], F32); t2 = self.sb(es, "da_t2", [128, 512], F32)
            va = self.sb(es, "da_va", [128, NT, 257], BF16)
            PT = [self.sb(es, "da_pt%d" % i, [128, 512], BF16) for i in range(2)]
            o1 = self.sb(es, "da_o1", [128, 4, 256], F32)
            o2 = self.sb(es, "da_o2", [128, 256], F32)
            rs = self.sb(es, "da_rs1", [128, 1], F32)
            ss = self.sb(es, "da_ss", [128, 1], F32); junk = self.sb(es, "da_junk", [128, 256], F32)
            og = self.sb(es, "da_og", [128, 256], BF16)
            ogT = [self.sb(es, "da_ogT%d" % i, [128, TT], BF16) for i in range(2)]
            ngb = self.sb(es, "da_ngb", [128, 256], F32)
            lamb = self.sb(es, "da_lamb", [128, 4, 128], F32)
            lamt = self.sb(es, "da_lamt", [128, 2], F32)
            nlam = self.sb(es, "da_nlam", [128, 1], F32)
            fw.dma('sp', RC[:], self.I["rope_c"], W=['da_rc'])
            fw.dma('sp', RS[:], self.I["rope_s"], W=['da_rs'])
            fw.dma('sp', permt[:], self.I["perm"], W=['da_perm'])
            fw.dma('sp', ngb[:], self.I["diff_ng"][j:j + 1, :].partition_broadcast(128)[:, 0, :], W=['da_ngb'])
            fw.op('dve', lambda e: e.tensor_scalar(ngb[:], ngb[:], 1.0 - lam_init, None, ALU.mult), R=['da_ngb'], W=['da_ngb'])
            fw.dma('sp', lamb[:].rearrange("p a b -> p (a b)"),
                   self.I["diff_lam"].rearrange("(o a) b -> o (a b)", o=1).partition_broadcast(128)[:, 0, :], W=['da_lamb'])
            for a in range(2):
                fw.op('dve', lambda e, a=a: e.tensor_tensor(lamb[:, 2 * a, :], lamb[:, 2 * a, :], lamb[:, 2 * a + 1, :], ALU.mult), R=['da_lamb'], W=['da_lamb'])
                fw.op('dve', lambda e, a=a: e.tensor_reduce(lamt[:, a:a + 1], lamb[:, 2 * a, :], AX.X, ALU.add), R=['da_lamb'], W=['da_lamt'])
            fw.op('act', lambda e: e.activation(lamt[:], lamt[:], AF.Exp), R=['da_lamt'], W=['da_lamt'])
            fw.op('dve', lambda e: e.tensor_tensor(nlam[:], lamt[:, 1:2], lamt[:, 0:1], ALU.subtract), R=['da_lamt'], W=['da_nlam'])
            fw.op('dve', lambda e: e.tensor_scalar(nlam[:], nlam[:], -lam_init, None, ALU.add), R=['da_nlam'], W=['da_nlam'])
            fw.op('dve', lambda e: e.memset(va[:, :, 256:257], 1.0), W=['da_va'])
            blocks = [(0, 512), (512, 512), (1024, 512), (1536, 512), (2048, 256)]
            for h in range(8):
                fw.dma('pool', wp[:, :, 0:256], W_in[:, h * 256:(h + 1) * 256].rearrange("(k p) c -> p k c", p=128), W=['da_w'])
                fw.dma('pool', wp[:, :, 256:512], W_in[:, 4096 + h * 256:4096 + (h + 1) * 256].rearrange("(k p) c -> p k c", p=128), W=['da_w'])
                fw.dma('pool', wp[:, :, 512:768], W_in[:, 2048 + h * 256:2048 + (h + 1) * 256].rearrange("(k p) c -> p k c", p=128), W=['da_w'])
                for bi, (t0, bw) in enumerate(blocks):
                    fw.dma('sp', hb[:, :, 0:bw], self.HT.rearrange("k p t -> p k t")[:, :, t0:t0 + bw], R=['HT'], W=['da_hb'])
                    for g in range(4):
                        bk = self.banks[g % 2]
                        for k in range(KC):
                            fw.op('pe', lambda e, k=k: e.matmul(bk[0][:, 0:bw], lhsT=wp[:, k, g * 128:(g + 1) * 128], rhs=hb[:, k, 0:bw],
                                                                start=(k == 0), stop=(k == KC - 1)), R=['da_w', 'da_hb'], W=[bk[1]])
                        fw.op('act', lambda e: e.copy(zT[g][:, t0:t0 + bw], bk[0][:, 0:bw]), R=[bk[1]], W=['da_z%d' % g])
                    for tl in range(bw // 128):
                        i = t0 // 128 + tl
                        bk = self.banks[2 + (tl % 2)]
                        for k in range(KC):
                            fw.op('pe', lambda e, k=k: e.matmul(bk[0][:, 0:256], lhsT=hb[:, k, tl * 128:(tl + 1) * 128], rhs=wp[:, k, 512:768],
                                                                start=(k == 0), stop=(k == KC - 1)), R=['da_w', 'da_hb'], W=[bk[1]])
                        fw.op('dve', lambda e: e.tensor_copy(va[:, i, 0:256], bk[0][:, 0:256]), R=[bk[1]], W=['da_va'])
                for g in range(4):
                    kz = 'da_z%d' % g; kf = 'da_f%d' % g
                    fw.op('pool', lambda e: e.tensor_copy(fT[g][:, 0:256], zT[g][:, 0:256]), R=[kz], W=[kf])
                    for nb in range(4):
                        c0 = nb * 512
                        bk = self.banks[nb % 2]
                        fw.op('pe', lambda e: e.matmul(bk[0], lhsT=permt[:], rhs=zT[g][:, 256 + c0:256 + c0 + 512], start=True, stop=True),
                              R=['da_perm', kz], W=[bk[1]])
                        fw.op('dve', lambda e: e.tensor_tensor(t1[:], bk[0], RS[:, c0:c0 + 512], ALU.mult), R=[bk[1], 'da_rs'], W=['da_t1'])
                        fw.op('pool', lambda e: e.tensor_tensor(t2[:], zT[g][:, 256 + c0:256 + c0 + 512], RC[:, c0:c0 + 512], ALU.mult), R=[kz, 'da_rc'], W=['da_t2'])
                        fw.op('dve', lambda e: e.tensor_tensor(fT[g][:, 256 + c0:256 + c0 + 512], t1[:], t2[:], ALU.add), R=['da_t1', 'da_t2'], W=[kf])
                qblocks = [(0, 256, (0, 1))] + [(256 + nb * 512, 512, tuple(range(NT))) for nb in range(4)]
                npt = 0
                for (q0, qw, ktiles) in qblocks:
                    nqt = qw // 128
                    for a in range(2):
                        KT, kK = fT[a], 'da_f%d' % a
                        QT, kQ = fT[2 + a], 'da_f%d' % (2 + a)
                        for ki, kt_ in enumerate(ktiles):
                            pb = npt % 2; npt += 1
                            bs = self.banks[pb]
                            fw.op('pe', lambda e: e.matmul(bs[0][:, 0:qw], lhsT=KT[:, kt_ * 128:(kt_ + 1) * 128], rhs=QT[:, q0:q0 + qw], start=True, stop=True),
                                  R=[kK, kQ], W=[bs[1]])
                            fw.op('act', lambda e: e.activation(PT[pb][:, 0:qw], bs[0][:, 0:qw], AF.Exp, scale=scale), R=[bs[1]], W=['da_pt%d' % pb])
                            for qt_ in range(nqt):
                                bo = self.banks[4 + qt_]
                                fw.op('pe', lambda e: e.matmul(bo[0][:, 0:257], lhsT=PT[pb][:, qt_ * 128:(qt_ + 1) * 128], rhs=va[:, kt_, :],
                                                               start=(ki == 0), stop=(ki == len(ktiles) - 1)), R=['da_pt%d' % pb, 'da_va'], W=[bo[1]])
                        for qt_ in range(nqt):
                            bo = self.banks[4 + qt_]
                            i = q0 // 128 + qt_
                            fw.op('dve', lambda e: e.reciprocal(rs[:], bo[0][:, 256:257]), R=[bo[1]], W=['da_rs1'])
                            if a == 0:
                                fw.op('dve', lambda e: e.tensor_scalar(o1[:, qt_, :], bo[0][:, 0:256], rs[:, 0:1], None, ALU.mult), R=[bo[1], 'da_rs1'], W=['da_o1'])
                            else:
                                fw.op('dve', lambda e: e.tensor_scalar(o2[:], bo[0][:, 0:256], rs[:, 0:1], None, ALU.mult), R=[bo[1], 'da_rs1'], W=['da_o2'])
                                fw.op('dve', lambda e: e.scalar_tensor_tensor(o2[:], o2[:], nlam[:, 0:1], o1[:, qt_, :], ALU.mult, ALU.add),
                                      R=['da_o2', 'da_o1', 'da_nlam'], W=['da_o2'])
                                fw.op('act', lambda e: e.activation(junk[:], o2[:], AF.Square, accum_out=ss[:]), R=['da_o2'], W=['da_ss', 'da_junk'])
                                fw.op('act', lambda e: e.activation(ss[:], ss[:], AF.Sqrt, bias=self.epsc[:], scale=1.0 / 256), R=['da_ss', 'eps'], W=['da_ss'])
                                fw.op('dve', lambda e: e.reciprocal(ss[:], ss[:]), R=['da_ss'], W=['da_ss'])
                                fw.op('dve', lambda e: e.scalar_tensor_tensor(og[:], o2[:], ss[:, 0:1], ngb[:], ALU.mult, ALU.mult), R=['da_o2', 'da_ss', 'da_ngb'], W=['da_og'])
                                for c in range(2):
                                    bt = self.banks[2 + c]
                                    pt = bt[0].bitcast(BF16)
                                    fw.op('pe', lambda e: e.transpose(pt[:, 0:128], og[:, c * 128:(c + 1) * 128], self.identb[:]), R=['da_og', 'identb'], W=[bt[1]])
                                    fw.op('act', lambda e: e.copy(ogT[c][:, i * 128:(i + 1) * 128], pt[:, 0:128]), R=[bt[1]], W=['da_ogT%d' % c])
                for c in range(2):
                    fw.dma('sp', self.OGT[2 * h + c], ogT[c][:], R=['da_ogT%d' % c], W=['OGT'])
            fw.barrier()

    def mlstm(self, layer):
        fw = self.fw
        j = layer // 3
        W_in = self.I["mlstm_w_in"][j]
        CAP = 15.0
        with ExitStack() as es:
            G = self.sb(es, "ml_G", [32, TT], F32); LF = self.sb(es, "ml_LF", [32, TT], F32)
            gb = self.sb(es, "ml_gb", [32, 1], F32)
            gsel = self.sb(es, "ml_gsel", [32, 32 * 128], F32)
            kTf = self.sb(es, "ml_kTf", [128, TT], BF16); qTf = self.sb(es, "ml_qTf", [128, TT], BF16)
            self.rmask = self.sb(es, "ml_rmask", [128, TT], BF16)
            va = self.sb(es, "ml_va", [128, NT, 257], BF16)
            sgo = self.sb(es, "ml_sgo", [128, NT, 256], BF16)
            hacc = self.sb(es, "ml_hacc", [128, NT, 256], F32)
            ogT = [self.sb(es, "ml_ogT%d" % i, [128, TT], BF16) for i in range(2)]
            ngb = self.sb(es, "ml_ngb", [128, 256], F32)
            ss = self.sb(es, "ml_ss", [128, NT], F32); junk = self.sb(es, "ml_junk", [128, 256], F32)
            dn = self.sb(es, "ml_dn", [128, 1], F32)
            og = [self.sb(es, "ml_og%d" % i, [128, 256], BF16) for i in range(2)]
            fw.op('pool', lambda e: e.memset(self.rmask[:], 1.0), W=['rmask'])
            fw.op('pool', lambda e: e.memset(self.rmask[:].rearrange("p (n l) -> p n l", l=128)[:, :, 0:1], 0.0), W=['rmask'])
            fw.op('dve', lambda e: e.memset(va[:, :, 256:257], 1.0), W=['ml_va'])
            fw.dma('sp', gsel[:], self.I["gsel"], W=['ml_gsel'])
            fw.dma('sp', gb[:], self.I["mlstm_gb"], W=['ml_gb'])
            fw.op('dve', lambda e: e.tensor_scalar(gb[:], gb[:], 1.0 / CAP, None, ALU.mult), R=['ml_gb'], W=['ml_gb'])
            blocks = [(0, 512), (512, 512), (1024, 512), (1536, 512), (2048, 256)]
            with ExitStack() as es1:
                wgt = self.sb(es1, "ml_wgt", [128, KC, 32], BF16)
                hb = self.sb(es1, "ml_hb0", [128, KC, 512], BF16)
                fw.dma('pool', wgt[:], W_in[:, 3072:3104].rearrange("(k p) c -> p k c", p=128), W=['ml_wgt'])
                for (t0, bw) in blocks:
                    fw.dma('sp', hb[:, :, 0:bw], self.HT.rearrange("k p t -> p k t")[:, :, t0:t0 + bw], R=['HT'], W=['ml_hb0'])
                    bk = self.banks[0]
                    for k in range(KC):
                        fw.op('pe', lambda e, k=k: e.matmul(bk[0][0:32, 0:bw], lhsT=wgt[:, k, :], rhs=hb[:, k, 0:bw], start=(k == 0), stop=(k == KC - 1)),
                              R=['ml_wgt', 'ml_hb0'], W=[bk[1]])
                    fw.op('act', lambda e: e.activation(G[:, t0:t0 + bw], bk[0][0:32, 0:bw], AF.Tanh, bias=gb[:], scale=1.0 / CAP), R=[bk[1], 'ml_gb'], W=['ml_G'])
                fw.op('dve', lambda e: e.tensor_scalar(G[:], G[:], CAP, None, ALU.mult), R=['ml_G'], W=['ml_G'])
                fw.op('act', lambda e: e.activation(LF[:], G[:], AF.Exp, scale=-1.0), R=['ml_G'], W=['ml_LF'])
                fw.op('act', lambda e: e.activation(LF[:], LF[:], AF.Ln, bias=self.onec[0:32, :]), R=['ml_LF'], W=['ml_LF'])
                fw.op('dve', lambda e: e.tensor_scalar(LF[:], LF[:], -1.0, None, ALU.mult), R=['ml_LF'], W=['ml_LF'])
                fw.barrier()
            for h in range(8):
                with ExitStack() as es1:
                    wp = self.sb(es1, "ml_w%d" % h, [128, KC, 768], BF16)
                    hb = self.sb(es1, "ml_hb%d" % (h + 1), [128, KC, 512], BF16)
                    fw.dma('pool', wp[:, :, 0:128], W_in[:, h * 128:(h + 1) * 128].rearrange("(k p) c -> p k c", p=128), W=['ml_w'])
                    fw.dma('pool', wp[:, :, 128:256], W_in[:, 3104 + h * 128:3104 + (h + 1) * 128].rearrange("(k p) c -> p k c", p=128), W=['ml_w'])
                    fw.dma('pool', wp[:, :, 256:512], W_in[:, 1024 + h * 256:1024 + (h + 1) * 256].rearrange("(k p) c -> p k c", p=128), W=['ml_w'])
                    fw.dma('pool', wp[:, :, 512:768], W_in[:, 4128 + h * 256:4128 + (h + 1) * 256].rearrange("(k p) c -> p k c", p=128), W=['ml_w'])
                    fw.dma('sp', ngb[:], self.I["mlstm_ng"][0:1, h * 256:(h + 1) * 256].partition_broadcast(128)[:, 0, :], W=['ml_ngb'])
                    for (t0, bw) in blocks:
                        fw.dma('sp', hb[:, :, 0:bw], self.HT.rearrange("k p t -> p k t")[:, :, t0:t0 + bw], R=['HT'], W=['ml_hb'])
                        for g in range(2):
                            bk = self.banks[g]
                            for k in range(KC):
                                fw.op('pe', lambda e, k=k: e.matmul(bk[0][:, 0:bw], lhsT=wp[:, k, g * 128:(g + 1) * 128], rhs=hb[:, k, 0:bw],
                                                                    start=(k == 0), stop=(k == KC - 1)), R=['ml_w', 'ml_hb'], W=[bk[1]])
                            if g == 0:
                                fw.op('act', lambda e: e.copy(kTf[:, t0:t0 + bw], bk[0][:, 0:bw]), R=[bk[1]], W=['ml_kTf'])
                            else:
                                fw.op('act', lambda e: e.activation(qTf[:, t0:t0 + bw], bk[0][:, 0:bw], AF.Copy, scale=128 ** -0.5), R=[bk[1]], W=['ml_qTf'])
                        for tl in range(bw // 128):
                            i = t0 // 128 + tl
                            for g in range(2):
                                bk = self.banks[2 + g]
                                for k in range(KC):
                                    fw.op('pe', lambda e, k=k: e.matmul(bk[0][:, 0:256], lhsT=hb[:, k, tl * 128:(tl + 1) * 128], rhs=wp[:, k, 256 + g * 256:512 + g * 256],
                                                                        start=(k == 0), stop=(k == KC - 1)), R=['ml_w', 'ml_hb'], W=[bk[1]])
                                if g == 0:
                                    fw.op('dve', lambda e: e.tensor_copy(va[:, i, 0:256], bk[0][:, 0:256]), R=[bk[1]], W=['ml_va'])
                                else:
                                    fw.op('act', lambda e: e.activation(sgo[:, i, :], bk[0][:, 0:256], AF.Sigmoid), R=[bk[1]], W=['ml_sgo'])
                    fw.op('pool', lambda e: e.tensor_tensor(sgo[:], sgo[:], ngb[:].rearrange("p (o c) -> p o c", o=1).to_broadcast([128, NT, 256]), ALU.mult),
                          R=['ml_sgo', 'ml_ngb'], W=['ml_sgo'])
                    fw.barrier()
                for d in range(2):
                    with ExitStack() as es2:
                        IW = self.sb(es2, "ml_IW%d_%d" % (h, d), [128, TT], F32); LFb = self.sb(es2, "ml_LFb%d_%d" % (h, d), [128, TT], F32)
                        A = self.sb(es2, "ml_A%d_%d" % (h, d), [128, TT], F32); TMP = self.sb(es2, "ml_T%d_%d" % (h, d), [128, TT], F32)
                        self.dp_e1 = self.sb(es2, "ml_e1%d_%d" % (h, d), [128, TT], F32)
                        qt = self.sb(es2, "ml_qt%d_%d" % (h, d), [128, TT], BF16); kt = self.sb(es2, "ml_kt%d_%d" % (h, d), [128, TT], BF16)
                        khT = self.sb(es2, "ml_khT%d_%d" % (h, d), [128, TT], BF16); kh = self.sb(es2, "ml_kh%d_%d" % (h, d), [128, NT, 128], BF16)
                        emid = self.sb(es2, "ml_em%d_%d" % (h, d), [128, NT], F32); eend = self.sb(es2, "ml_ee%d_%d" % (h, d), [128, NT], F32)
                        ri = (2 * d) * 8 + h
                        rf = (2 * d + 1) * 8 + h
                        for (src, ksrc, r, dst, kdst) in ((G, 'ml_G', ri, IW, 'ml_IW'), (LF, 'ml_LF', rf, LFb, 'ml_LFb')):
                            for bi, (t0, bw) in enumerate(blocks):
                                bk = self.banks[bi % 2]
                                fw.op('pe', lambda e: e.matmul(bk[0][:, 0:bw], lhsT=gsel[:, r * 128:(r + 1) * 128], rhs=src[:, t0:t0 + bw], start=True, stop=True),
                                      R=['ml_gsel', ksrc], W=[bk[1]])
                                fw.op('act', lambda e: e.copy(dst[:, t0:t0 + bw], bk[0][:, 0:bw]), R=[bk[1]], W=[kdst])
                        pfx = 'ml'
                        self.decay_prep(pfx, d, LFb, A, TMP, qTf, kTf, qt, kt, khT, emid, eend,
                                        {'lf': 'ml_LFb', 'q': 'ml_qTf', 'k': 'ml_kTf', 'iw': 'ml_IW'}, iw=IW)
                        self.to_tokmajor(khT, pfx + 'khT', kh, pfx + 'kh', self.banks[0])

                        def post(i, ops, kops, d=d):
                            fw.op('dve', lambda e: e.tensor_scalar(dn[:], ops[:, 256:257], -1.0, None, ALU.mult), R=[kops], W=['ml_dn'])
                            fw.op('dve', lambda e: e.scalar_tensor_tensor(dn[:], ops[:, 256:257], 1.0, dn[:], ALU.max, ALU.max), R=[kops, 'ml_dn'], W=['ml_dn'])
                            fw.op('dve', lambda e: e.reciprocal(dn[:], dn[:]), R=['ml_dn'], W=['ml_dn'])
                            if d == 0:
                                fw.op('dve', lambda e: e.tensor_scalar(hacc[:, i, :], ops[:, 0:256], dn[:, 0:1], None, ALU.mult), R=[kops, 'ml_dn'], W=['ml_hacc'])
                            else:
                                fw.op('dve', lambda e: e.scalar_tensor_tensor(hacc[:, i, :], ops[:, 0:256], dn[:, 0:1], hacc[:, i, :], ALU.mult, ALU.add),
                                      R=[kops, 'ml_dn', 'ml_hacc'], W=['ml_hacc'])
                        self.gla(es2, pfx + 'g', 257, qt, kt, kh, va, emid, eend,
                                 {'q': pfx + 'qt', 'k': pfx + 'kt', 'kh': pfx + 'kh', 'v': 'ml_va', 'e': pfx + 'e'}, d, post)
                        fw.barrier()
                for i in range(NT):
                    fw.op('act', lambda e, i=i: e.activation(junk[:], hacc[:, i, :], AF.Square, accum_out=ss[:, i:i + 1]), R=['ml_hacc'], W=['ml_ss', 'ml_junk'])
                fw.op('act', lambda e: e.activation(ss[:], ss[:], AF.Sqrt, bias=self.epsc[:], scale=1.0 / 256), R=['ml_ss', 'eps'], W=['ml_ss'])
                fw.op('dve', lambda e: e.reciprocal(ss[:], ss[:]), R=['ml_ss'], W=['ml_ss'])
                for i in range(NT):
                    b = i % 2
                    fw.op('dve', lambda e, i=i, b=b: e.scalar_tensor_tensor(og[b][:], hacc[:, i, :], ss[:, i:i + 1], sgo[:, i, :], ALU.mult, ALU.mult),
                          R=['ml_hacc', 'ml_ss', 'ml_sgo'], W=['ml_og%d' % b])
                    for c in range(2):
                        bt = self.banks[b * 2 + c]
                        pt = bt[0].bitcast(BF16)
                        fw.op('pe', lambda e: e.transpose(pt[:, 0:128], og[b][:, c * 128:(c + 1) * 128], self.identb[:]), R=['ml_og%d' % b, 'identb'], W=[bt[1]])
                        fw.op('act', lambda e: e.copy(ogT[c][:, i * 128:(i + 1) * 128], pt[:, 0:128]), R=[bt[1]], W=['ml_ogT%d' % c])
                for c in range(2):
                    fw.dma('sp', self.OGT[2 * h + c], ogT[c][:], R=['ml_ogT%d' % c], W=['OGT'])
            fw.barrier()

    def phaseC(self, layer, W_out=None):
        fw = self.fw
        kind, j = layer % 3, layer // 3
        W_out = self.I[["hgrn_w_out", "diff_w_out", "mlstm_w_out"][kind]][j]
        full_ctx = layer < DEPTH - 1
        with ExitStack() as es:
            og = self.sb(es, "pc_og", [128, KC, TT], BF16)
            wb = [self.sb(es, "pc_w%d" % i, [128, KC, 512], BF16) for i in range(2)]
            g1 = self.sb(es, "pc_g1", [128, D], F32); g1c = self.sb(es, "pc_g1c", [128, D], F32)
            xt = [self.sb(es, "pc_x%d" % i, [128, 512], F32) for i in range(3)]
            for k in range(KC):
                fw.dma('sp', og[:, k, :], self.OGT[k], R=['OGT'], W=['pc_og'])
            fw.dma('sp', g1[:], self.MOD[0:1, 2 * D:3 * D].partition_broadcast(128)[:, 0, :], R=['MOD'], W=['pc_g1'])
            fw.dma('sp', g1c[:], self.MOD[1:2, 2 * D:3 * D].partition_broadcast(128)[:, 0, :], R=['MOD'], W=['pc_g1c'])
            n = 0
            for nb in range(4):
                w = wb[nb % 2]; kw = 'pc_w%d' % (nb % 2)
                fw.dma('pool', w[:], W_out[:, nb * 512:(nb + 1) * 512].rearrange("(k p) f -> p k f", p=128), W=[kw])
                for i in range(NT):
                    if i < 2 and not full_ctx:
                        continue
                    b = n % 3; n += 1
                    x_, kx = xt[b], 'pc_x%d' % b
                    src, ksrc = self.src_rows(layer, i)
                    fw.dma('sp', x_[:], src[:, nb * 512:(nb + 1) * 512], R=[ksrc], W=[kx])
                    bk = self.banks[n % 8]
                    for k in range(KC):
                        fw.op('pe', lambda e, k=k: e.matmul(bk[0], lhsT=og[:, k, i * 128:(i + 1) * 128], rhs=w[:, k, :],
                                                            start=(k == 0), stop=(k == KC - 1)), R=['pc_og', kw], W=[bk[1]])
                    gg, kg = (g1, 'pc_g1') if i >= 2 else (g1c, 'pc_g1c')
                    fw.op('dve', lambda e: e.tensor_tensor(bk[0], bk[0], gg[:, nb * 512:(nb + 1) * 512], ALU.mult), R=[bk[1], kg], W=[bk[1]])
                    fw.op('dve', lambda e: e.tensor_tensor(x_[:], x_[:], bk[0], ALU.add), R=[bk[1], kx], W=[kx])
                    rows = slice(i * 128, (i + 1) * 128)
                    cols = slice(nb * 512, (nb + 1) * 512)
                    fw.dma('sp', self.XA[rows, cols], x_[:], R=[kx], W=['XA'])
                    fw.dma('sp', self.XB[rows, cols], x_[:], R=[kx], W=['XB%d' % i])
            fw.barrier()

    def moe(self, layer):
        fw = self.fw
        nc = self.nc
        full_ctx = layer < DEPTH - 1
        ntile = NT if full_ctx else NT
        with ExitStack() as eso:
            idxTi = self.sb(eso, "mo_idxTi", [128, 3, NE], I32)
            gT = self.sb(eso, "mo_gT", [128, 3, NE], F32)
            with ExitStack() as es:
                xt = [self.sb(es, "mr_x%d" % i, [128, D], F32) for i in range(2)]
                h32 = [self.sb(es, "mr_h%d" % i, [128, KC, 128], F32) for i in range(2)]
                ss = self.sb(es, "mr_ss", [128, 1], F32); sd = self.sb(es, "mr_sd", [128, 1], F32)
                junk = self.sb(es, "mr_junk", [128, D], BF16)
                wr = self.sb(es, "mr_wr", [128, KC, NE], F32)
                affT = self.sb(es, "mr_affT", [NE, TT], F32)
                ex = self.sb(es, "mr_ex", [128, NE], F32); mx = self.sb(es, "mr_mx", [128, 1], F32); sm = self.sb(es, "mr_sm", [128, 1], F32)
                vals = self.sb(es, "mr_vals", [NE, 288], F32); idx = self.sb(es, "mr_idx", [NE, 288], U32)
                idxf = self.sb(es, "mr_idxf", [NE, 288], F32)
                tT = self.sb(es, "mr_tT", [128, 2, NE], F32)
                fw.dma('sp', wr[:], self.I["moe_router"][layer].rearrange("(k p) e -> p k e", p=128), W=['mr_wr'])
                for i in range(NT):
                    if i < 2 and not full_ctx:
                        continue
                    b = i % 2; kx = 'mr_x%d' % b; kh = 'mr_h%d' % b
                    fw.dma('sp', xt[b][:], self.XA[i * 128:(i + 1) * 128, :], W=[kx])
                    self.norm_tile(xt[b], kx, 128, 4 if i >= 2 else 6, h32[b], kh, (ss, sd, junk), 'mr_t',
                                   bank_ids=(0, 1, 2, 3))
                    pl = self.banks[4 + b]
                    for k in range(KC):
                        fw.op('pe', lambda e, k=k: e.matmul(pl[0][:, 0:NE], lhsT=h32[b][:, k, :], rhs=wr[:, k, :], start=(k == 0), stop=(k == KC - 1)),
                              R=[kh, 'mr_wr'], W=[pl[1]])
                    fw.op('dve', lambda e: e.tensor_reduce(mx[:], pl[0][:, 0:NE], AX.X, ALU.max, negate=True), R=[pl[1]], W=['mr_mx'])
                    fw.op('act', lambda e: e.activation(ex[:], pl[0][:, 0:NE], AF.Exp, bias=mx[:], accum_out=sm[:]), R=[pl[1], 'mr_mx'], W=['mr_ex', 'mr_sm'])
                    fw.op('dve', lambda e: e.reciprocal(sm[:], sm[:]), R=['mr_sm'], W=['mr_sm'])
                    fw.op('dve', lambda e: e.tensor_scalar(ex[:], ex[:], sm[:, 0:1], None, ALU.mult), R=['mr_ex', 'mr_sm'], W=['mr_ex'])
                    pt = self.banks[6 + b]
                    fw.op('pe', lambda e: e.transpose(pt[0][0:NE, 0:128], ex[:], self.ident[:]), R=['mr_ex', 'ident'], W=[pt[1]])
                    fw.op('dve', lambda e: e.tensor_copy(affT[:, i * 128:(i + 1) * 128], pt[0][0:NE, 0:128]), R=[pt[1]], W=['mr_affT'])
                def topk(work, nround, o0):
                    for r in range(nround):
                        sl = slice(o0 + r * 8, o0 + (r + 1) * 8)
                        fw.op('dve', lambda e: e.max(vals[:, sl], work), R=['mr_affT'], W=['mr_vals'])
                        fw.op('dve', lambda e: e.max_index(idx[:, sl], vals[:, sl], work), R=['mr_affT', 'mr_vals'], W=['mr_idx'])
                        fw.op('dve', lambda e: e.match_replace(work, vals[:, sl], work, -1.0), R=['mr_vals', 'mr_idx'], W=['mr_affT'])
                topk(affT[:, 256:TT], 32, 0)
                if full_ctx:
                    topk(affT[:, 0:256], 4, 256)
                else:
                    fw.op('dve', lambda e: e.memset(vals[:, 256:288], 0.0), W=['mr_vals'])
                    fw.op('dve', lambda e: e.memset(idx[:, 256:288], 0), W=['mr_idx'])
                fw.op('dve', lambda e: e.tensor_copy(idxf[:], idx[:]), R=['mr_idx'], W=['mr_idxf'])
                fw.op('dve', lambda e: e.tensor_scalar(idxf[:, 0:256], idxf[:, 0:256], 256.0, None, ALU.add), R=['mr_idxf'], W=['mr_idxf'])
                for (src, ksrc, dst, kdst) in ((idxf, 'mr_idxf', idxTi, 'mo_idxTi'), (vals, 'mr_vals', gT, 'mo_gT')):
                    pt = self.banks[0]
                    for st in range(2):
                        fw.op('pe', lambda e, st=st: e.transpose(pt[0][:, st * NE:(st + 1) * NE], src[:, st * 128:(st + 1) * 128], self.ident[0:NE, 0:NE]),
                              R=[ksrc, 'ident'], W=[pt[1]])
                    fw.op('pe', lambda e: e.transpose(pt[0][0:32, 2 * NE:3 * NE], src[:, 256:288], self.ident[0:NE, 0:NE]), R=[ksrc, 'ident'], W=[pt[1]])
                    fw.op('dve', lambda e: e.tensor_copy(dst[:, 0:2, :].rearrange("p a b -> p (a b)"), pt[0][:, 0:2 * NE]), R=[pt[1]], W=[kdst])
                    fw.op('dve', lambda e: e.tensor_copy(dst[0:32, 2, :], pt[0][0:32, 2 * NE:3 * NE]), R=[pt[1]], W=[kdst])
                fw.barrier()
            with ExitStack() as es:
                nst = 3 if full_ctx else 2
                NS = 288 if full_ctx else 256
                xg = [self.sb(es, "me_xg%d" % i, [128, D], F32) for i in range(3)]
                junk = self.sb(es, "me_junk", [128, D], BF16)
                ss = self.sb(es, "me_ss", [128, 1], F32); sd = self.sb(es, "me_sd", [128, 1], F32)
                xsT = self.sb(es, "me_xsT", [128, KC, 288], BF16)
                wg = [self.sb(es, "me_wg%d" % i, [128, KC, 256], BF16) for i in range(4)]
                wu = [self.sb(es, "me_wu%d" % i, [128, KC, 256], BF16) for i in range(4)]
                wd = self.sb(es, "me_wd", [128, NF, D], BF16)
                hmT = self.sb(es, "me_hmT", [128, NF, 288], BF16)
                sg = [self.sb(es, "me_sg%d" % i, [128, 288], F32) for i in range(2)]
                yo = [self.sb(es, "me_yo%d" % i, [128, D], F32) for i in range(2)]
                g2b = self.sb(es, "me_g2b", [128, D], F32); g2cb = self.sb(es, "me_g2cb", [128, D], F32)
                fw.dma('sp', g2b[:], self.MOD[0:1, 5 * D:6 * D].partition_broadcast(128)[:, 0, :], W=['me_g2b'])
                fw.dma('sp', g2cb[:], self.MOD[1:2, 5 * D:6 * D].partition_broadcast(128)[:, 0, :], W=['me_g2cb'])
                Wg, Wu, Wd = self.I["moe_w_gate"][layer], self.I["moe_w_up"][layer], self.I["moe_w_down"][layer]
                nyo = 0
                xsTs = [xsT, self.sb(es, "me_xsT2", [128, KC, 288], BF16)]

                def gather(ex_):
                    for st in range(nst):
                        n = 128 if st < 2 else 32
                        fw.dma('pool', None, None, R=['mo_idxTi', 'XA'], W=['me_xg%d' % st],
                               fn=lambda g, st=st, n=n: g.indirect_dma_start(
                                   out=xg[st][0:n, :], out_offset=None, in_=self.XA[:, :],
                                   in_offset=bass.IndirectOffsetOnAxis(ap=idxTi[0:n, st, ex_:ex_ + 1], axis=0)))

                def norm1(ex_, st):
                    xs_ = xsTs[ex_ % 2]
                    n = 128 if st < 2 else 32
                    self.norm_tile(xg[st], 'me_xg%d' % st, n, 4 if st < 2 else 6, xs_[:, :, st * 128:st * 128 + n], 'me_xsT%d' % (ex_ % 2),
                                   (ss, sd, junk), 'me_t', bank_ids=(4, 5, 6, 7))

                pieces = [(e_, pc) for e_ in range(NE) for pc in range(6)]
                issued = [0]

                def issue_upto(m):
                    while issued[0] < min(m, len(pieces)):
                        e_, pc = pieces[issued[0]]
                        b = issued[0] % 4
                        f0 = pc * 256
                        fwd_ = min(256, FF - f0)
                        fw.dma('pool', wg[b][:, :, 0:fwd_], Wg[e_][:, f0:f0 + fwd_].rearrange("(k p) f -> p k f", p=128), W=['me_wg%d' % b])
                        fw.dma('pool', wu[b][:, :, 0:fwd_], Wu[e_][:, f0:f0 + fwd_].rearrange("(k p) f -> p k f", p=128), W=['me_wu%d' % b])
                        issued[0] += 1
                gather(0)
                for st in range(nst):
                    norm1(0, st)
                issue_upto(3)
                for ex_ in range(NE):
                    xs_ = xsTs[ex_ % 2]; kxs = 'me_xsT%d' % (ex_ % 2)
                    if ex_ + 1 < NE:
                        gather(ex_ + 1)
                    ft = 0
                    for pc in range(6):
                        pi = ex_ * 6 + pc
                        issue_upto(pi + 4)
                        if pc == 1:
                            fw.dma('pool', wd[:], Wd[ex_].rearrange("(c p) d -> p c d", p=128), W=['me_wd'])
                        f0 = pc * 256
                        fwd_ = min(256, FF - f0)
                        b = pi % 4
                        for fl in range(fwd_ // 128):
                            pb = ft % 2
                            pg, pu = self.banks[pb * 2], self.banks[pb * 2 + 1]
                            for (w_, kw_, p_) in ((wg[b], 'me_wg%d' % b, pg), (wu[b], 'me_wu%d' % b, pu)):
                                for k in range(KC):
                                    fw.op('pe', lambda e, k=k: e.matmul(p_[0][:, 0:NS], lhsT=w_[:, k, fl * 128:(fl + 1) * 128], rhs=xs_[:, k, 0:NS],
                                                                        start=(k == 0), stop=(k == KC - 1)), R=[kw_, kxs], W=[p_[1]])
                            fw.op('act', lambda e: e.activation(sg[pb][:, 0:NS], pg[0][:, 0:NS], AF.Silu), R=[pg[1]], W=['me_sg%d' % pb])
                            fw.op('dve', lambda e: e.tensor_tensor(hmT[:, ft, 0:NS], sg[pb][:, 0:NS], pu[0][:, 0:NS], ALU.mult),
                                  R=['me_sg%d' % pb, pu[1]], W=['me_hmT'])
                            ft += 1
                        if ex_ + 1 < NE and 2 <= pc < 2 + nst:
                            norm1(ex_ + 1, pc - 2)
                    for st in range(nst):
                        n = 128 if st < 2 else 32
                        for nb in range(4):
                            bk = self.banks[4 + nb]
                            for c in range(NF):
                                fw.op('pe', lambda e, c=c: e.matmul(bk[0][0:n, :], lhsT=hmT[:, c, st * 128:st * 128 + n], rhs=wd[:, c, nb * 512:(nb + 1) * 512],
                                                                    start=(c == 0), stop=(c == NF - 1)), R=['me_hmT', 'me_wd'], W=[bk[1]])
                        yb = nyo % 2; nyo += 1
                        gg, kg = (g2b, 'me_g2b') if st < 2 else (g2cb, 'me_g2cb')
                        fw.op('dve', lambda e: e.scalar_tensor_tensor(yo[yb][0:n, :], self.P1[0:n, :], gT[0:n, st, ex_:ex_ + 1], gg[0:n, :], ALU.mult, ALU.mult),
                              R=['pb4', 'pb5', 'pb6', 'pb7', 'mo_gT', kg], W=['me_yo%d' % yb])
                        fw.dma('pool', None, None, R=['me_yo%d' % yb, 'mo_idxTi'], W=['XBs'],
                               fn=lambda g, st=st, n=n, yb=yb: g.indirect_dma_start(
                                   out=self.XB[:, :], out_offset=bass.IndirectOffsetOnAxis(ap=idxTi[0:n, st, ex_:ex_ + 1], axis=0),
                                   in_=yo[yb][0:n, :], in_offset=None, compute_op=ALU.add))
                fw.barrier()

    def final_norm(self):
        fw = self.fw
        with ExitStack() as es:
            xt = [self.sb(es, "fn_x%d" % i, [128, D], F32) for i in range(2)]
            fg = self.sb(es, "fn_g", [128, D], F32)
            ss = self.sb(es, "fn_ss", [128, 1], F32); junk = self.sb(es, "fn_junk", [128, D], BF16)
            epsc = self.sb(es, "fn_eps", [128, 1], F32)
            fw.op('dve', lambda e: e.memset(epsc[:], EPS), W=['fn_eps'])
            fw.dma('sp', fg[:], self.I["final_g"].partition_broadcast(128)[:, 0, :], W=['fn_g'])
            for i in range(2, NT):
                b = i % 2; kx = 'fn_x%d' % b
                fw.dma('sp', xt[b][:], self.XB[i * 128:(i + 1) * 128, :], R=['XB'], W=[kx])
                fw.op('act', lambda e: e.activation(junk[:], xt[b][:], AF.Square, accum_out=ss[:]), R=[kx], W=['fn_ss'])
                fw.op('act', lambda e: e.activation(ss[:], ss[:], AF.Sqrt, bias=epsc[:], scale=1.0 / D), R=['fn_ss', 'fn_eps'], W=['fn_ss'])
                fw.op('dve', lambda e: e.reciprocal(ss[:], ss[:]), R=['fn_ss'], W=['fn_ss'])
                fw.op('dve', lambda e: e.scalar_tensor_tensor(xt[b][:], xt[b][:], ss[:, 0:1], fg[:], ALU.mult, ALU.mult), R=[kx, 'fn_ss', 'fn_g'], W=[kx])
                fw.dma('sp', self.out[(i - 2) * 128:(i - 1) * 128, :], xt[b][:], R=[kx])


def make_consts():
    c = {}
    c["ident"] = np.eye(128, dtype=np.float32)
    s = np.arange(128)[:, None]; t = np.arange(128)[None, :]
    c["mask_f"] = (t >= s).astype(np.uint8)
    c["mask_b"] = (t <= s).astype(np.uint8)
    T = 2048; GW = 64
    pos = np.arange(T)
    row = (pos // GW).astype(np.float32); col = (pos % GW).astype(np.float32)
    half = 64
    inv = (1.0 / (np.float32(10000.0) ** (np.arange(0, half, 2, dtype=np.float32) / np.float32(half)))).astype(np.float32)
    ar = row[None, :] * inv[:, None]
    ac = col[None, :] * inv[:, None]
    C = np.concatenate([np.cos(ar), np.cos(ar), np.cos(ac), np.cos(ac)], axis=0)
    S = np.concatenate([-np.sin(ar), np.sin(ar), -np.sin(ac), np.sin(ac)], axis=0)
    c["rope_c"] = C.astype(np.float32); c["rope_s"] = S.astype(np.float32)
    perm = np.zeros((128, 128), np.float32)
    for m in range(128):
        blk = m // 32
        sw = (blk ^ 1) * 32 + (m % 32)
        perm[sw, m] = 1.0
    c["perm"] = perm
    gsel = np.zeros((32, 32, 128), np.float32)
    for r in range(32):
        gsel[r, r, :] = 1.0
    c["gsel"] = gsel.reshape(32, 32 * 128)
    return c

def prep_shared(inp):
    d = {}
    for k in ["ada_w", "ada_b", "hgrn_w_in", "hgrn_w_out", "diff_w_in", "diff_w_out", "mlstm_w_in", "mlstm_w_out",
              "moe_router", "moe_w_gate", "moe_w_up", "moe_w_down"]:
        d[k] = np.ascontiguousarray(inp[k])
    d["norm_g"] = np.ascontiguousarray(inp["norm_g"]).reshape(8, 2048)
    d["final_g"] = np.ascontiguousarray(inp["final_g"]).reshape(1, 2048)
    d["hgrn_lb"] = np.ascontiguousarray(inp["hgrn_lb_logits"]).reshape(4, 2048)
    d["hgrn_ng"] = np.ascontiguousarray(inp["hgrn_norm_g"]).reshape(2, 128)
    d["diff_lam"] = np.ascontiguousarray(inp["diff_lambda"]).reshape(4, 128)
    d["diff_ng"] = np.ascontiguousarray(inp["diff_norm_g"]).reshape(1, 256)
    d["mlstm_gb"] = np.ascontiguousarray(inp["mlstm_gate_b"]).reshape(32, 1)
    d["mlstm_ng"] = np.ascontiguousarray(inp["mlstm_norm_g"]).reshape(1, 2048)
    d.update(make_consts())
    return d

def prep_core(inp, b):
    return {"x": np.ascontiguousarray(inp["x"][b]), "ctx": np.ascontiguousarray(inp["ctx"][b]),
            "cc": np.ascontiguousarray(np.stack([inp["c"][b], inp["c_ctx"]], axis=0))}


_CACHE = {}


def kernel(**inputs):
    inputs = {k: np.asarray(v) for k, v in inputs.items()}
    if "nc" not in _CACHE:
        bld = Builder(nlayers=DEPTH, dbg=False)
        _CACHE["nc"] = bld.build()
        _CACHE["used"] = set(bld.I.keys())
    nc = _CACHE["nc"]
    used = _CACHE["used"]
    shared = {k: v for k, v in prep_shared(inputs).items() if k in used}
    n = 8
    in_maps = []
    for b in range(n):
        m = dict(shared)
        m.update(prep_core(inputs, b))
        in_maps.append(m)
    res = run_bass_kernel_spmd(nc, in_maps, core_ids=list(range(n)))
    out = np.stack([np.asarray(res.results[b]["out"]) for b in range(n)], axis=0)
    return out.astype(np.float32, copy=False)
```

```python
import numpy as np
from contextlib import ExitStack
import concourse.bass as bass
import concourse.mybir as mybir
from concourse.bass_utils import run_bass_kernel_spmd

F32 = mybir.dt.float32
F32R = mybir.dt.float32r
BF16 = mybir.dt.bfloat16
I32 = mybir.dt.int32
U32 = mybir.dt.uint32
U8 = mybir.dt.uint8
AF = mybir.ActivationFunctionType
ALU = mybir.AluOpType
AX = mybir.AxisListType


class FW:
    def __init__(self, nc, es, n_dma_sems=40):
        self.nc = nc
        self.es = es
        self.E = {'pe': nc.tensor, 'dve': nc.vector, 'act': nc.scalar,
                  'pool': nc.gpsimd, 'sp': nc.sync}
        self.esem = {k: es.enter_context(nc.semaphore('s_' + k)) for k in self.E}
        self.ecnt = {k: 0 for k in self.E}
        self.waited = {k: {} for k in self.E}
        self.lastw = {}
        self.readers = {}
        self.dsem = [es.enter_context(nc.semaphore('d%d' % i)) for i in range(n_dma_sems)]
        self.dcnt = [0] * n_dma_sems
        self.drr = 0
        self.nwaits = 0
        self.ninst = 0

    def _deps(self, R, W):
        deps = []
        for k in R:
            t = self.lastw.get(k)
            if t is not None:
                deps.append(t)
        for k in W:
            t = self.lastw.get(k)
            if t is not None:
                deps.append(t)
            rd = self.readers.get(k)
            if rd:
                deps.extend(rd.values())
        return deps

    def _wait(self, eng, deps):
        w = self.waited[eng]
        best = {}
        for (sem, val, deng, sid) in deps:
            if deng == eng and eng == 'pe':
                continue
            if w.get(sid, 0) >= val:
                continue
            if sid not in best or best[sid][1] < val:
                best[sid] = (sem, val)
        for sid, (sem, val) in best.items():
            self.E[eng].wait_ge(sem, val)
            w[sid] = val
            self.nwaits += 1

    def _commit(self, tok, R, W):
        for k in W:
            self.lastw[k] = tok
            self.readers[k] = {}
        for k in R:
            self.readers.setdefault(k, {})[tok[3]] = tok

    def op(self, eng, fn, R=(), W=()):
        self._wait(eng, self._deps(R, W))
        inst = fn(self.E[eng])
        self.ecnt[eng] += 1
        inst.then_inc(self.esem[eng], 1)
        self.ninst += 1
        tok = (self.esem[eng], self.ecnt[eng], eng, 'e_' + eng)
        self._commit(tok, R, W)
        return inst

    def dma(self, q, out, in_, R=(), W=(), fn=None, **kw):
        deps = self._deps(R, W)
        i = self.drr
        self.drr = (self.drr + 1) % len(self.dsem)
        if self.dcnt[i] > 0:
            deps.append((self.dsem[i], 16 * self.dcnt[i], 'dma', 'd%d' % i))
        self._wait(q, deps)
        if fn is None:
            inst = self.E[q].dma_start(out=out, in_=in_, **kw)
        else:
            inst = fn(self.E[q])
        self.dcnt[i] += 1
        inst.then_inc(self.dsem[i], 16)
        self.ninst += 1
        tok = (self.dsem[i], 16 * self.dcnt[i], 'dma', 'd%d' % i)
        self._commit(tok, R, W)
        return inst

    def barrier(self):
        deps = []
        for k in self.E:
            if self.ecnt[k] > 0:
                deps.append((self.esem[k], self.ecnt[k], 'x', 'e_' + k))
        for i, s in enumerate(self.dsem):
            if self.dcnt[i] > 0:
                deps.append((s, 16 * self.dcnt[i], 'dma', 'd%d' % i))
        for k in self.E:
            self._wait(k, deps)
        self.lastw.clear()
        self.readers.clear()

    def final_wait(self, eng='sp'):
        deps = []
        for k in self.E:
            if self.ecnt[k] > 0:
                deps.append((self.esem[k], self.ecnt[k], 'x', 'e_' + k))
        for i, s in enumerate(self.dsem):
            if self.dcnt[i] > 0:
                deps.append((s, 16 * self.dcnt[i], 'dma', 'd%d' % i))
        self._wait(eng, deps)


T_CTX = 256
T_LAT = 2048
TT = 2304
NT = 18
D = 2048
KC = 16
EPS = 1e-6
DEPTH = 4
NE = 16
FF = 1408
NF = 11


class Builder:
    def __init__(self, nlayers=4, dbg=False, stop_after=None):
        self.nlayers = nlayers
        self.dbg = dbg
        self.stop_after = stop_after
        nc = bass.Bass("TRN2", target_bir_lowering=False)
        self.nc = nc
        self.shapes = {}
        def inp(name, shape, dt=F32):
            self.shapes[name] = (list(shape), dt)
        inp("x", [T_LAT, D]); inp("ctx", [T_CTX, D]); inp("cc", [2, D])
        inp("ada_w", [DEPTH, D, 6 * D]); inp("ada_b", [DEPTH, 6 * D]); inp("norm_g", [8, D]); inp("final_g", [1, D])
        inp("hgrn_w_in", [2, D, 10240]); inp("hgrn_lb", [4, D]); inp("hgrn_ng", [2, 128]); inp("hgrn_w_out", [2, D, D])
        inp("diff_w_in", [1, D, 6144]); inp("diff_lam", [4, 128]); inp("diff_ng", [1, 256]); inp("diff_w_out", [1, D, D])
        inp("mlstm_w_in", [1, D, 6176]); inp("mlstm_gb", [32, 1]); inp("mlstm_ng", [1, D]); inp("mlstm_w_out", [1, D, D])
        inp("moe_router", [DEPTH, D, NE]); inp("moe_w_gate", [DEPTH, NE, D, FF]); inp("moe_w_up", [DEPTH, NE, D, FF])
        inp("moe_w_down", [DEPTH, NE, FF, D])
        inp("ident", [128, 128]); inp("mask_f", [128, 128], U8); inp("mask_b", [128, 128], U8)
        inp("rope_c", [128, T_LAT]); inp("rope_s", [128, T_LAT]); inp("perm", [128, 128]); inp("gsel", [32, 32 * 128])
        outer = self
        class LazyIn(dict):
            def __missing__(d, name):
                shape, dt = outer.shapes[name]
                ap = nc.dram_tensor(name, shape, dt, kind="ExternalInput").ap()
                d[name] = ap
                return ap
        self.I = LazyIn()
        self.out = nc.dram_tensor("out", [T_LAT, D], F32, kind="ExternalOutput").ap()
        if dbg:
            self.dbg_x = nc.dram_tensor("dbg_x", [TT, D], F32, kind="ExternalOutput").ap()
        self.XA = nc.dram_tensor("XA", [TT, D], F32).ap()
        self.XB = nc.dram_tensor("XB", [TT, D], F32).ap()
        self.HT = nc.dram_tensor("HT", [KC, 128, TT], BF16).ap()
        self.OGT = nc.dram_tensor("OGT", [KC, 128, TT], BF16).ap()
        self.MOD = nc.dram_tensor("MOD", [2, 6 * D], F32).ap()

    def sb(self, es, name, shape, dt):
        self.nsb = getattr(self, 'nsb', 0) + 1
        return es.enter_context(self.nc.sbuf_tensor("sb%d_%s" % (self.nsb, name), list(shape), dt))

    def src_rows(self, layer, i):
        if layer == 0:
            if i < 2:
                return self.I["ctx"][i * 128:(i + 1) * 128, :], 'in_ctx'
            return self.I["x"][(i - 2) * 128:(i - 1) * 128, :], 'in_x'
        return self.XB[i * 128:(i + 1) * 128, :], 'XB%d' % i

    def rows2cols(self, src, R, n, dst, bank, keyR, keyW):
        fw = self.fw
        pk = bank[1]
        pt = bank[0]
        for j in range(n):
            fw.op('pe', lambda e, j=j: e.transpose(pt[:, j * R:(j + 1) * R], src[0:R, j * 128:(j + 1) * 128],
                                                   self.ident[0:R, 0:R]), R=[keyR, 'ident'], W=[pk])
        fw.op('dve', lambda e: e.tensor_copy(dst[:].rearrange("p n r -> p (n r)"), pt[:, 0:n * R]), R=[pk], W=[keyW])

    def build(self):
        nc = self.nc
        with ExitStack() as es:
            self.fw = fw = FW(nc, es)
            self.ident = self.sb(es, "ident", [128, 128], F32)
            self.identb = self.sb(es, "identb", [128, 128], BF16)
            self.maskf = self.sb(es, "maskf", [128, 128], U8)
            self.maskb = self.sb(es, "maskb", [128, 128], U8)
            self.zerob = self.sb(es, "zerob", [128, 128], BF16)
            self.S2 = self.sb(es, "S2", [128, KC, 2], BF16)
            self.ngcol = self.sb(es, "ngcol", [128, KC, 8], F32)
            self.modcol = self.sb(es, "modcol", [128, 96, 2], F32)
            self.AB = self.sb(es, "AB", [128, 8, KC], F32)
            self.epsc = self.sb(es, "epsc", [128, 1], F32)
            self.onec = self.sb(es, "onec", [128, 1], F32)
            self.lbcol = self.sb(es, "lbcol", [128, KC, 4], F32)
            self.lb1 = self.sb(es, "lb1", [128, KC, 2], F32)
            self.oml1 = self.sb(es, "oml1", [128, KC, 2], F32)
            self.P0 = es.enter_context(nc.psum_tensor("P0", [128, 2048], F32))
            self.P1 = es.enter_context(nc.psum_tensor("P1", [128, 2048], F32))
            self.banks = [(self.P0[:, j * 512:(j + 1) * 512], 'pb%d' % j) for j in range(4)] + \
                         [(self.P1[:, j * 512:(j + 1) * 512], 'pb%d' % (4 + j)) for j in range(4)]
            fw.dma('sp', self.ident[:], self.I["ident"], W=['ident'])
            fw.dma('sp', self.maskf[:], self.I["mask_f"], W=['maskf'])
            fw.dma('sp', self.maskb[:], self.I["mask_b"], W=['maskb'])
            fw.op('dve', lambda e: e.tensor_copy(self.identb[:], self.ident[:]), R=['ident'], W=['identb'])
            fw.op('dve', lambda e: e.memset(self.zerob[:], 0.0), W=['zerob'])
            fw.op('dve', lambda e: e.memset(self.epsc[:], EPS), W=['eps'])
            fw.op('dve', lambda e: e.memset(self.onec[:], 1.0), W=['onec'])
            self.prologue()
            for layer in range(self.nlayers):
                self.adaln(layer)
                self.phaseA(layer)
                kind = layer % 3
                if kind == 0:
                    self.hgrn(layer)
                elif kind == 1:
                    self.diffattn(layer)
                else:
                    self.mlstm(layer)
                self.phaseC(layer)
                if self.dbg:
                    o = nc.dram_tensor("dbg_xm%d" % layer, [TT, D], F32, kind="ExternalOutput").ap()
                    for i in range(NT):
                        fw.dma('sp', o[i * 128:(i + 1) * 128, :], self.XB[i * 128:(i + 1) * 128, :])
                    fw.barrier()
                if self.stop_after == ('mix', layer):
                    break
                self.moe(layer)
                if self.dbg:
                    o = nc.dram_tensor("dbg_x%d" % layer, [TT, D], F32, kind="ExternalOutput").ap()
                    for i in range(NT):
                        fw.dma('sp', o[i * 128:(i + 1) * 128, :], self.XB[i * 128:(i + 1) * 128, :])
                    fw.barrier()
            if self.dbg:
                fw.barrier()
                for nm, t in (("MOD", self.MOD), ("HT", self.HT), ("OGT", self.OGT), ("XA", self.XA)):
                    o = nc.dram_tensor("dbg_" + nm, list(t.shape), t.dtype, kind="ExternalOutput").ap()
                    fw.dma('sp', o, t)
                for i in range(NT):
                    fw.dma('sp', self.dbg_x[i * 128:(i + 1) * 128, :], self.XB[i * 128:(i + 1) * 128, :], R=['XB'])
            else:
                self.final_norm()
            fw.final_wait('sp')
        return nc

    def prologue(self):
        fw = self.fw
        with ExitStack() as es:
            rows = self.sb(es, "pr_rows", [8, D], F32)
            cols = self.sb(es, "pr_cols", [128, KC, 8], F32)
            fw.dma('sp', rows[:], self.I["norm_g"], W=['pr_rows'])
            self.rows2cols(rows, 8, KC, self.ngcol, self.banks[0], 'pr_rows', 'ngcol')
            fw.dma('sp', rows[0:2, :], self.I["cc"], W=['pr_rows'])
            c2 = self.sb(es, "pr_c2", [128, KC, 2], F32)
            self.rows2cols(rows, 2, KC, c2, self.banks[1], 'pr_rows', 'pr_c2')
            fw.op('act', lambda e: e.activation(self.S2[:], c2[:], AF.Silu), R=['pr_c2'], W=['S2'])
            fw.dma('sp', rows[0:4, :], self.I["hgrn_lb"], W=['pr_rows'])
            self.rows2cols(rows, 4, KC, self.lbcol, self.banks[2], 'pr_rows', 'lbcol')
            ex = self.sb(es, "pr_ex", [128, KC, 4], F32)
            fw.op('act', lambda e: e.activation(ex[:], self.lbcol[:], AF.Exp), R=['lbcol'], W=['pr_ex'])
            sm = self.sb(es, "pr_sm", [128, KC, 2], F32)
            fw.op('dve', lambda e: e.tensor_tensor(sm[:], ex[:, :, 0:2], ex[:, :, 2:4], ALU.add), R=['pr_ex'], W=['pr_sm'])
            fw.op('dve', lambda e: e.reciprocal(sm[:], sm[:]), R=['pr_sm'], W=['pr_sm'])
            fw.op('dve', lambda e: e.tensor_tensor(self.lb1[:], ex[:, :, 2:4], sm[:], ALU.mult), R=['pr_ex', 'pr_sm'], W=['lb1'])
            fw.op('dve', lambda e: e.tensor_scalar(self.oml1[:], self.lb1[:], -1.0, 1.0, ALU.mult, ALU.add), R=['lb1'], W=['oml1'])
            fw.barrier()

    def adaln(self, layer):
        fw = self.fw
        nc = self.nc
        with ExitStack() as es:
            wb = [self.sb(es, "ad_w%d" % i, [128, KC, 512], BF16) for i in range(2)]
            modrow = self.sb(es, "ad_mod", [2, 6 * D], F32)
            bias = self.sb(es, "ad_b", [2, 6 * D], F32)
            fw.dma('sp', bias[:], self.I["ada_b"][layer:layer + 1, :].partition_broadcast(2)[:, 0, :], W=['ad_b'])
            for nb in range(24):
                b = nb % 2
                src = self.I["ada_w"][layer, :, nb * 512:(nb + 1) * 512].rearrange("(k p) f -> p k f", p=128)
                fw.dma('pool', wb[b][:], src, W=['ad_w%d' % b])
                bk = self.banks[nb % 4]
                for k in range(KC):
                    fw.op('pe', lambda e, k=k: e.matmul(bk[0][0:2, :], lhsT=self.S2[:, k, :], rhs=wb[b][:, k, :],
                                                        start=(k == 0), stop=(k == KC - 1)),
                          R=['S2', 'ad_w%d' % b], W=[bk[1]])
                fw.op('dve', lambda e: e.tensor_tensor(modrow[:, nb * 512:(nb + 1) * 512], bk[0][0:2, :],
                                                       bias[:, nb * 512:(nb + 1) * 512], ALU.add),
                      R=[bk[1], 'ad_b'], W=['ad_mod'])
            fw.dma('sp', self.MOD, modrow[:], R=['ad_mod'], W=['MOD'])
            self.rows2cols(modrow, 2, 96, self.modcol, self.banks[4], 'ad_mod', 'modcol')
            mc = self.modcol
            AB = self.AB

            def seg(s, r):
                return mc[:, 16 * s:16 * s + 16, r]
            for which in range(2):
                g = self.ngcol[:, :, layer * 2 + which]
                for r in range(2):
                    ia = which * 4 + r * 2
                    sh, sc = seg(3 * which, r), seg(3 * which + 1, r)
                    fw.op('dve', lambda e, ia=ia, sc=sc, g=g: e.scalar_tensor_tensor(AB[:, ia, :], sc, 1.0, g, ALU.add, ALU.mult),
                          R=['modcol', 'ngcol'], W=['AB'])
                    fw.op('dve', lambda e, ia=ia, sh=sh: e.tensor_copy(AB[:, ia + 1, :], sh), R=['modcol'], W=['AB'])
            fw.barrier()

    def norm_tile(self, xt, kx, nrows, ia, dst, kdst, tmp, ktmp, bank_ids=(0, 1, 2, 3), dst_f32=None):
        fw = self.fw
        ss, sd, junk = tmp
        n = nrows
        fw.op('act', lambda e: e.activation(junk[0:n, :], xt[0:n, :], AF.Square, accum_out=ss[0:n, 0:1]), R=[kx], W=[ktmp])
        fw.op('act', lambda e: e.activation(sd[0:n, :], ss[0:n, :], AF.Sqrt, bias=self.epsc[0:n, :], scale=1.0 / D), R=[ktmp, 'eps'], W=[ktmp + 'd'])
        fw.op('dve', lambda e: e.reciprocal(sd[0:n, :], sd[0:n, :]), R=[ktmp + 'd'], W=[ktmp + 'd'])
        fw.op('dve', lambda e: e.tensor_scalar(xt[0:n, :], xt[0:n, :], sd[0:n, 0:1], None, ALU.mult), R=[kx, ktmp + 'd'], W=[kx])
        for q in range(4):
            bk = self.banks[bank_ids[q]]
            for j in range(4):
                k = q * 4 + j
                fw.op('pe', lambda e, k=k, j=j: e.transpose(bk[0][:, j * 128:j * 128 + n], xt[0:n, k * 128:(k + 1) * 128],
                                                            self.ident[0:n, 0:n]), R=[kx, 'ident'], W=[bk[1]])
            for j in range(4):
                k = q * 4 + j
                fw.op('act', lambda e, k=k, j=j: e.activation(dst[:, k, 0:n], bk[0][:, j * 128:j * 128 + n], AF.Identity,
                                                              bias=self.AB[:, ia + 1, k:k + 1], scale=self.AB[:, ia, k:k + 1]),
                      R=[bk[1], 'AB'], W=[kdst])

    def phaseA(self, layer):
        fw = self.fw
        with ExitStack() as es:
            xt = [self.sb(es, "pa_x%d" % i, [128, D], F32) for i in range(2)]
            ht = [self.sb(es, "pa_h%d" % i, [128, KC, 128], BF16) for i in range(2)]
            ss = self.sb(es, "pa_ss", [128, 1], F32); sd = self.sb(es, "pa_sd", [128, 1], F32)
            junk = self.sb(es, "pa_junk", [128, D], BF16)
            for i in range(NT):
                b = i % 2
                src, ksrc = self.src_rows(layer, i)
                fw.dma('sp', xt[b][:], src, R=[ksrc], W=['pa_x%d' % b])
                self.norm_tile(xt[b], 'pa_x%d' % b, 128, 0 if i >= 2 else 2, ht[b], 'pa_h%d' % b, (ss, sd, junk), 'pa_t',
                               bank_ids=(0, 1, 2, 3) if b == 0 else (4, 5, 6, 7))
                fw.dma('sp', self.HT.rearrange("k p t -> p k t")[:, :, i * 128:(i + 1) * 128], ht[b][:], R=['pa_h%d' % b], W=['HT'])
            fw.barrier()

    def gla(self, es, pfx, E, qt, kt, kh, vt, emid, eend, keys, direction, post):
        fw = self.fw
        if isinstance(es, tuple):
            S, Sb, at = es
        else:
            self.uid = getattr(self, 'uid', 0) + 1
            pfx = pfx + '_%d' % self.uid
            S = self.sb(es, pfx + "_S", [128, E], F32)
            Sb = self.sb(es, pfx + "_Sb", [128, E], BF16)
            at = [self.sb(es, pfx + "_at%d" % i, [128, 128], BF16) for i in range(2)]
        kS, kSb = pfx + '_S', pfx + '_Sb'
        fw.op('dve', lambda e: e.memset(S[:], 0.0), W=[kS])
        for b_ in range(2):
            fw.op('dve', lambda e: e.memset(at[b_][:], 0.0), W=[pfx + '_at%d' % b_])
        mask, kmask = (self.maskf, 'maskf') if direction == 0 else (self.maskb, 'maskb')
        if direction == 0:
            order = list(range(NT))
        else:
            order = [1, 0] + list(range(NT - 1, 1, -1))
        def pre(n):
            i = order[n]; b = n % 2
            cs = slice(i * 128, (i + 1) * 128)
            pa, pd = self.banks[2 + b], self.banks[6 + b]
            fw.op('pe', lambda e: e.matmul(pa[0][:, 0:128], lhsT=kt[:, cs], rhs=qt[:, cs], start=True, stop=True),
                  R=[keys['k'], keys['q']], W=[pa[1]])
            fw.op('dve', lambda e: e.copy_predicated(at[b][:], mask[:], pa[0][:, 0:128]),
                  R=[pa[1], kmask], W=[pfx + '_at%d' % b])
            fw.op('pe', lambda e: e.matmul(pd[0][:, 0:E], lhsT=kh[:, i, :], rhs=vt[:, i, :], start=True, stop=True),
                  R=[keys['kh'], keys['v']], W=[pd[1]])
        pre(0)
        for n, i in enumerate(order):
            b = n % 2
            cs = slice(i * 128, (i + 1) * 128)
            po, pd = self.banks[4 + b], self.banks[6 + b]
            fw.op('act', lambda e: e.activation(Sb[:], S[:], AF.Identity, scale=emid[:, i:i + 1]), R=[kS, keys['e']], W=[kSb])
            if n + 1 < len(order):
                pre(n + 1)
            fw.op('pe', lambda e: e.matmul(po[0][:, 0:E], lhsT=qt[:, cs], rhs=Sb[:], start=True, stop=False),
                  R=[keys['q'], kSb], W=[po[1]])
            fw.op('pe', lambda e: e.matmul(po[0][:, 0:E], lhsT=at[b][:], rhs=vt[:, i, :], start=False, stop=True),
                  R=[pfx + '_at%d' % b, keys['v']], W=[po[1]])
            post(i, po[0][:, 0:E], po[1])
            fw.op('dve', lambda e: e.scalar_tensor_tensor(S[:], S[:], eend[:, i:i + 1], pd[0][:, 0:E], ALU.mult, ALU.add),
                  R=[kS, keys['e'], pd[1]], W=[kS])

    def decay_prep(self, pfx, direction, lf, a, tmp, q, k, qt, kt, khT, emid, eend, kin, iw=None):
        fw = self.fw
        a3 = a[:].rearrange("p (n l) -> p n l", l=128)
        t3 = tmp[:].rearrange("p (n l) -> p n l", l=128)
        if direction == 0:
            fw.op('dve', lambda e: e.tensor_tensor_scan(a[:], self.rmask[:], lf[:], 0.0, ALU.mult, ALU.add), R=[kin['lf'], 'rmask'], W=[pfx + 'a'])
            endpos = 127
        else:
            fw.op('dve', lambda e: e.tensor_tensor_scan(a[:, ::-1], self.rmask[:], lf[:, ::-1], 0.0, ALU.mult, ALU.add), R=[kin['lf'], 'rmask'], W=[pfx + 'a'])
            endpos = 0
        mid = a3[:, :, 64:65]
        end = a3[:, :, endpos:endpos + 1]
        fw.op('act', lambda e: e.activation(emid[:].rearrange("p (n o) -> p n o", o=1), mid, AF.Exp), R=[pfx + 'a'], W=[pfx + 'e'])
        fw.op('act', lambda e: e.activation(eend[:].rearrange("p (n o) -> p n o", o=1), end, AF.Exp), R=[pfx + 'a'], W=[pfx + 'e'])
        fw.op('dve', lambda e: e.tensor_tensor(t3, a3, mid.to_broadcast([128, NT, 128]), ALU.subtract), R=[pfx + 'a'], W=[pfx + 't'])
        fw.op('dve', lambda e: e.tensor_scalar(tmp[:], tmp[:], 80.0, -80.0, ALU.min, ALU.max), R=[pfx + 't'], W=[pfx + 't'])
        e1 = self.dp_e1
        fw.op('act', lambda e: e.activation(e1[:], tmp[:], AF.Exp), R=[pfx + 't'], W=['dp_e1'])
        fw.op('dve', lambda e: e.tensor_tensor(qt[:], q[:], e1[:], ALU.mult), R=[kin['q'], 'dp_e1'], W=[pfx + 'qt'])
        if iw is not None:
            fw.op('dve', lambda e: e.tensor_tensor(tmp[:], tmp[:], iw[:], ALU.subtract), R=[pfx + 't', kin['iw']], W=[pfx + 't'])
        fw.op('act', lambda e: e.activation(e1[:], tmp[:], AF.Exp, scale=-1.0), R=[pfx + 't'], W=['dp_e1'])
        fw.op('dve', lambda e: e.tensor_tensor(kt[:], k[:], e1[:], ALU.mult), R=[kin['k'], 'dp_e1'], W=[pfx + 'kt'])
        fw.op('dve', lambda e: e.tensor_tensor(t3, a3, end.to_broadcast([128, NT, 128]), ALU.subtract), R=[pfx + 'a'], W=[pfx + 't'])
        if iw is not None:
            fw.op('dve', lambda e: e.tensor_tensor(tmp[:], tmp[:], iw[:], ALU.subtract), R=[pfx + 't', kin['iw']], W=[pfx + 't'])
        fw.op('act', lambda e: e.activation(e1[:], tmp[:], AF.Exp, scale=-1.0), R=[pfx + 't'], W=['dp_e1'])
        fw.op('dve', lambda e: e.tensor_tensor(khT[:], k[:], e1[:], ALU.mult), R=[kin['k'], 'dp_e1'], W=[pfx + 'khT'])

    def to_tokmajor(self, srcT, ksrc, dst, kdst, bank):
        fw = self.fw
        for g in range(0, NT, 4):
            n = min(4, NT - g)
            pt = bank[0].bitcast(BF16)
            for j in range(n):
                i = g + j
                fw.op('pe', lambda e, i=i, j=j: e.transpose(pt[:, j * 128:(j + 1) * 128], srcT[:, i * 128:(i + 1) * 128], self.identb[:]),
                      R=[ksrc, 'identb'], W=[bank[1]])
            fw.op('act', lambda e, g=g, n=n: e.copy(dst[:, g:g + n, :].rearrange("p n c -> p (n c)"), pt[:, 0:n * 128]), R=[bank[1]], W=[kdst])

    def hgrn(self, layer):
        fw = self.fw
        j = layer // 3
        W_in = self.I["hgrn_w_in"][j]
        with ExitStack() as es:
            wp = self.sb(es, "hg_w", [128, KC, 5, 128], BF16)
            hb = [self.sb(es, "hg_hb%d" % i, [128, KC, 512], BF16) for i in range(2)]
            EF = [[self.sb(es, "hg_ef%d_%d" % (p, d), [128, TT], F32) for d in range(2)] for p in range(2)]
            Q = [self.sb(es, "hg_q%d" % p, [128, TT], BF16) for p in range(2)]
            vt = [self.sb(es, "hg_v%d" % p, [128, NT, 128], BF16) for p in range(2)]
            gs = [self.sb(es, "hg_g%d" % p, [128, NT, 128], BF16) for p in range(2)]
            Kf = self.sb(es, "hg_k", [128, TT], BF16)
            A = self.sb(es, "hg_a", [128, TT], F32)
            TMP = self.sb(es, "hg_tmp", [128, TT], F32)
            self.dp_e1 = self.sb(es, "dp_e1", [128, TT], F32)
            self.rmask = self.sb(es, "rmask", [128, TT], BF16)
            qt = self.sb(es, "hg_qt", [128, TT], BF16); kt = self.sb(es, "hg_kt", [128, TT], BF16)
            khT = self.sb(es, "hg_khT", [128, TT], BF16); kh = self.sb(es, "hg_kh", [128, NT, 128], BF16)
            oacc = self.sb(es, "hg_o", [128, NT, 128], F32)
            emid = self.sb(es, "hg_emid", [128, NT], F32); eend = self.sb(es, "hg_eend", [128, NT], F32)
            ngb = self.sb(es, "hg_ngb", [128, 128], F32)
            ss = self.sb(es, "hg_ss", [128, NT], F32); junk = self.sb(es, "hg_junk", [128, 128], F32)
            og = [self.sb(es, "hg_og%d" % i, [128, 128], BF16) for i in range(2)]
            ogT = self.sb(es, "hg_ogT", [128, TT], BF16)
            gS = self.sb(es, "hg_S", [128, 128], F32); gSb = self.sb(es, "hg_Sb", [128, 128], BF16)
            gat = [self.sb(es, "hg_at%d" % i, [128, 128], BF16) for i in range(2)]
            fw.op('pool', lambda e: e.memset(self.rmask[:], 1.0), W=['rmask'])
            fw.op('pool', lambda e: e.memset(self.rmask[:].rearrange("p (n l) -> p n l", l=128)[:, :, 0:1], 0.0), W=['rmask'])
            fw.dma('sp', ngb[:], self.I["hgrn_ng"][j:j + 1, :].partition_broadcast(128)[:, 0, :], W=['hg_ngb'])
            blocks = [(0, 512), (512, 512), (1024, 512), (1536, 512), (2048, 256)]
            nhb = [0]

            def load_w(hh):
                for g in range(5):
                    src = W_in[:, g * 2048 + hh * 128: g * 2048 + (hh + 1) * 128].rearrange("(k p) c -> p k c", p=128)
                    fw.dma('pool', wp[:, :, g, :], src, W=['hg_w'])

            def inproj(hh, bis):
                p = hh % 2
                w = wp; kw = 'hg_w'
                for bi in bis:
                    t0, bw = blocks[bi]
                    hbi = nhb[0] % 2; nhb[0] += 1
                    h = hb[hbi]; kh_ = 'hg_hb%d' % hbi
                    fw.dma('sp', h[:, :, 0:bw], self.HT.rearrange("k p t -> p k t")[:, :, t0:t0 + bw], R=['HT'], W=[kh_])
                    for gi, g in enumerate((0, 1, 3)):
                        bk = self.banks[gi % 2]
                        for k in range(KC):
                            fw.op('pe', lambda e, k=k, g=g: e.matmul(bk[0][:, 0:bw], lhsT=w[:, k, g, :], rhs=h[:, k, 0:bw],
                                                                     start=(k == 0), stop=(k == KC - 1)), R=[kw, kh_], W=[bk[1]])
                        if g == 3:
                            fw.op('act', lambda e: e.activation(Q[p][:, t0:t0 + bw], bk[0][:, 0:bw], AF.Silu), R=[bk[1]], W=['hg_q%d' % p])
                        else:
                            fw.op('act', lambda e, g=g: e.activation(EF[p][g][:, t0:t0 + bw], bk[0][:, 0:bw], AF.Exp, scale=-1.0),
                                  R=[bk[1]], W=['hg_ef%d_%d' % (p, g)])
                    for tl in range(bw // 128):
                        i = t0 // 128 + tl
                        for gi, g in enumerate((2, 4)):
                            bk = self.banks[2 + gi]
                            for k in range(KC):
                                fw.op('pe', lambda e, k=k, g=g: e.matmul(bk[0][:, 0:128], lhsT=h[:, k, tl * 128:(tl + 1) * 128], rhs=w[:, k, g, :],
                                                                         start=(k == 0), stop=(k == KC - 1)), R=[kw, kh_], W=[bk[1]])
                            if g == 2:
                                fw.op('dve', lambda e: e.tensor_copy(vt[p][:, i, :], bk[0][:, 0:128]), R=[bk[1]], W=['hg_v%d' % p])
                            else:
                                fw.op('act', lambda e: e.activation(gs[p][:, i, :], bk[0][:, 0:128], AF.Silu), R=[bk[1]], W=['hg_g%d' % p])
                if bis[-1] == 4:
                    fw.op('pool', lambda e: e.tensor_tensor(gs[p][:], gs[p][:], ngb[:].rearrange("p (o c) -> p o c", o=1).to_broadcast([128, NT, 128]), ALU.mult),
                          R=['hg_g%d' % p, 'hg_ngb'], W=['hg_g%d' % p])
                    if hh + 1 < 16:
                        load_w(hh + 1)

            load_w(0)
            inproj(0, [0, 1, 2, 3, 4])
            for hh in range(16):
                p = hh % 2
                for d in range(2):
                    ef = EF[p][d]; kef = 'hg_ef%d_%d' % (p, d)
                    fw.op('dve', lambda e: e.tensor_scalar(ef[:], ef[:], 1.0, None, ALU.add), R=[kef], W=[kef])
                    fw.op('dve', lambda e: e.reciprocal(ef[:], ef[:]), R=[kef], W=[kef])
                    if j == 1:
                        fw.op('dve', lambda e: e.tensor_scalar(ef[:], ef[:], self.oml1[:, hh, d:d + 1], self.lb1[:, hh, d:d + 1], ALU.mult, ALU.add),
                              R=[kef, 'lb1', 'oml1'], W=[kef])
                    fw.op('dve', lambda e: e.tensor_scalar(Kf[:], ef[:], -1.0, 1.0, ALU.mult, ALU.add), R=[kef], W=['hg_k'])
                    fw.op('act', lambda e: e.activation(ef[:], ef[:], AF.Ln), R=[kef], W=[kef])
                    pfx = 'hg'
                    self.decay_prep(pfx, d, ef, A, TMP, Q[p], Kf, qt, kt, khT, emid, eend, {'lf': kef, 'q': 'hg_q%d' % p, 'k': 'hg_k'})
                    if hh + 1 < 16:
                        inproj(hh + 1, [0, 1] if d == 0 else [2, 3, 4])
                    self.to_tokmajor(khT, pfx + 'khT', kh, pfx + 'kh', self.banks[0])

                    def post(i, ops, kops, d=d):
                        if d == 0:
                            fw.op('act', lambda e: e.copy(oacc[:, i, :], ops), R=[kops], W=['hg_o'])
                        else:
                            fw.op('dve', lambda e: e.tensor_tensor(oacc[:, i, :], oacc[:, i, :], ops, ALU.add), R=[kops, 'hg_o'], W=['hg_o'])
                    self.gla((gS, gSb, gat), pfx + 'g', 128, qt, kt, kh, vt[p], emid, eend,
                             {'q': pfx + 'qt', 'k': pfx + 'kt', 'kh': pfx + 'kh', 'v': 'hg_v%d' % p, 'e': pfx + 'e'}, d, post)
                for i in range(NT):
                    fw.op('act', lambda e, i=i: e.activation(junk[:], oacc[:, i, :], AF.Square, accum_out=ss[:, i:i + 1]), R=['hg_o'], W=['hg_ss', 'hg_junk'])
                fw.op('act', lambda e: e.activation(ss[:], ss[:], AF.Sqrt, bias=self.epsc[:], scale=1.0 / 128), R=['hg_ss', 'eps'], W=['hg_ss'])
                fw.op('dve', lambda e: e.reciprocal(ss[:], ss[:]), R=['hg_ss'], W=['hg_ss'])
                for i in range(NT):
                    b = i % 2
                    fw.op('dve', lambda e, i=i, b=b: e.scalar_tensor_tensor(og[b][:], oacc[:, i, :], ss[:, i:i + 1], gs[p][:, i, :], ALU.mult, ALU.mult),
                          R=['hg_o', 'hg_ss', 'hg_g%d' % p], W=['hg_og%d' % b])
                    bk = self.banks[b]
                    pt = bk[0].bitcast(BF16)
                    fw.op('pe', lambda e, b=b: e.transpose(pt[:, 0:128], og[b][:], self.identb[:]), R=['hg_og%d' % b, 'identb'], W=[bk[1]])
                    fw.op('act', lambda e, i=i: e.copy(ogT[:, i * 128:(i + 1) * 128], pt[:, 0:128]), R=[bk[1]], W=['hg_ogT'])
                fw.dma('sp', self.OGT[hh], ogT[:], R=['hg_ogT'], W=['OGT'])
            fw.barrier()

    def diffattn(self, layer):
        import math
        fw = self.fw
        j = layer // 3
        W_in = self.I["diff_w_in"][j]
        lam_init = 0.8 - 0.6 * math.exp(-0.3 * layer)
        scale = 128 ** -0.5
        with ExitStack() as es:
            wp = self.sb(es, "da_w", [128, KC, 768], BF16)
            hb = self.sb(es, "da_hb", [128, KC, 512], BF16)
            zT = [self.sb(es, "da_z%d" % i, [128, TT], F32) for i in range(4)]
            fT = [self.sb(es, "da_f%d" % i, [128, TT], BF16) for i in range(4)]
            RC = self.sb(es, "da_rc", [128, T_LAT], F32); RS = self.sb(es, "da_rs", [128, T_LAT], F32)
            permt = self.sb(es, "da_perm", [128, 128], F32)
            t1 = self.sb(es, "da_t1", [128, 512], F32); t2 = self.sb(es, "da_t2", [128, 512], F32)
            va = self.sb(es, "da_va", [128, NT, 257], BF16)
            PT = [self.sb(es, "da_pt%d" % i, [128, 512], BF16) for i in range(2)]
            o1 = self.sb(es, "da_o1", [128, 4, 256], F32)
            o2 = self.sb(es, "da_o2", [128, 256], F32)
            rs = self.sb(es, "da_rs1", [128, 1], F32)
            ss = self.sb(es, "da_ss", [128, 1], F32); junk = self.sb(es, "da_junk", [128, 256], F32)
            og = self.sb(es, "da_og", [128, 256], BF16)
            ogT = [self.sb(es, "da_ogT%d" % i, [128, TT], BF16) for i in range(2)]
            ngb = self.sb(es, "da_ngb", [128, 256], F32)
            lamb = self.sb(es, "da_lamb", [128, 4, 128], F32)
            lamt = self.sb(es, "da_lamt", [128, 2], F32)
            nlam = self.sb(es, "da_nlam", [128, 1], F32)
            fw.dma('sp', RC[:], self.I["rope_c"], W=['da_rc'])
            fw.dma('sp', RS[:], self.I["rope_s"], W=['da_rs'])
            fw.dma('sp', permt[:], self.I["perm"], W=['da_perm'])
            fw.dma('sp', ngb[:], self.I["diff_ng"][j:j + 1, :].partition_broadcast(128)[:, 0, :], W=['da_ngb'])
            fw.op('dve', lambda e: e.tensor_scalar(ngb[:], ngb[:], 1.0 - lam_init, None, ALU.mult), R=['da_ngb'], W=['da_ngb'])
            fw.dma('sp', lamb[:].rearrange("p a b -> p (a b)"),
                   self.I["diff_lam"].rearrange("(o a) b -> o (a b)", o=1).partition_broadcast(128)[:, 0, :], W=['da_lamb'])
            for a in range(2):
                fw.op('dve', lambda e, a=a: e.tensor_tensor(lamb[:, 2 * a, :], lamb[:, 2 * a, :], lamb[:, 2 * a + 1, :], ALU.mult), R=['da_lamb'], W=['da_lamb'])
                fw.op('dve', lambda e, a=a: e.tensor_reduce(lamt[:, a:a + 1], lamb[:, 2 * a, :], AX.X, ALU.add), R=['da_lamb'], W=['da_lamt'])
            fw.op('act', lambda e: e.activation(lamt[:], lamt[:], AF.Exp), R=['da_lamt'], W=['da_lamt'])
            fw.op('dve', lambda e: e.tensor_tensor(nlam[:], lamt[:, 1:2], lamt[:, 0:1], ALU.subtract), R=['da_lamt'], W=['da_nlam'])
            fw.op('dve', lambda e: e.tensor_scalar(nlam[:], nlam[:], -lam_init, None, ALU.add), R=['da_nlam'], W=['da_nlam'])
            fw.op('dve', lambda e: e.memset(va[:, :, 256:257], 1.0), W=['da_va'])
            blocks = [(0, 512), (512, 512), (1024, 512), (1536, 512), (2048, 256)]
            for h in range(8):
                fw.dma('pool', wp[:, :, 0:256], W_in[:, h * 256:(h + 1) * 256].rearrange("(k p) c -> p k c", p=128), W=['da_w'])
                fw.dma('pool', wp[:, :, 256:512], W_in[:, 4096 + h * 256:4096 + (h + 1) * 256].rearrange("(k p) c -> p k c", p=128), W=['da_w'])
                fw.dma('pool', wp[:, :, 512:768], W_in[:, 2048 + h * 256:2048 + (h + 1) * 256].rearrange("(k p) c -> p k c", p=128), W=['da_w'])
                for bi, (t0, bw) in enumerate(blocks):
                    fw.dma('sp', hb[:, :, 0:bw], self.HT.rearrange("k p t -> p k t")[:, :, t0:t0 + bw], R=['HT'], W=['da_hb'])
                    for g in range(4):
                        bk = self.banks[g % 2]
                        for k in range(KC):
                            fw.op('pe', lambda e, k=k: e.matmul(bk[0][:, 0:bw], lhsT=wp[:, k, g * 128:(g + 1) * 128], rhs=hb[:, k, 0:bw],
                                                                start=(k == 0), stop=(k == KC - 1)), R=['da_w', 'da_hb'], W=[bk[1]])
                        fw.op('act', lambda e: e.copy(zT[g][:, t0:t0 + bw], bk[0][:, 0:bw]), R=[bk[1]], W=['da_z%d' % g])
                    for tl in range(bw // 128):
                        i = t0 // 128 + tl
                        bk = self.banks[2 + (tl % 2)]
                        for k in range(KC):
                            fw.op('pe', lambda e, k=k: e.matmul(bk[0][:, 0:256], lhsT=hb[:, k, tl * 128:(tl + 1) * 128], rhs=wp[:, k, 512:768],
                                                                start=(k == 0), stop=(k == KC - 1)), R=['da_w', 'da_hb'], W=[bk[1]])
                        fw.op('dve', lambda e: e.tensor_copy(va[:, i, 0:256], bk[0][:, 0:256]), R=[bk[1]], W=['da_va'])
                for g in range(4):
                    kz = 'da_z%d' % g; kf = 'da_f%d' % g
                    fw.op('pool', lambda e: e.tensor_copy(fT[g][:, 0:256], zT[g][:, 0:256]), R=[kz], W=[kf])
                    for nb in range(4):
                        c0 = nb * 512
                        bk = self.banks[nb % 2]
                        fw.op('pe', lambda e: e.matmul(bk[0], lhsT=permt[:], rhs=zT[g][:, 256 + c0:256 + c0 + 512], start=True, stop=True),
                              R=['da_perm', kz], W=[bk[1]])
                        fw.op('dve', lambda e: e.tensor_tensor(t1[:], bk[0], RS[:, c0:c0 + 512], ALU.mult), R=[bk[1], 'da_rs'], W=['da_t1'])
                        fw.op('pool', lambda e: e.tensor_tensor(t2[:], zT[g][:, 256 + c0:256 + c0 + 512], RC[:, c0:c0 + 512], ALU.mult), R=[kz, 'da_rc'], W=['da_t2'])
                        fw.op('dve', lambda e: e.tensor_tensor(fT[g][:, 256 + c0:256 + c0 + 512], t1[:], t2[:], ALU.add), R=['da_t1', 'da_t2'], W=[kf])
                qblocks = [(0, 256, (0, 1))] + [(256 + nb * 512, 512, tuple(range(NT))) for nb in range(4)]
                npt = 0
                for (q0, qw, ktiles) in qblocks:
                    nqt = qw // 128
                    for a in range(2):
                        KT, kK = fT[a], 'da_f%d' % a
                        QT, kQ = fT[2 + a], 'da_f%d' % (2 + a)
                        def score(ki_):
                            kt2 = ktiles[ki_]
                            pb2 = (npt + ki_) % 2
                            bs = self.banks[pb2]
                            fw.op('pe', lambda e: e.matmul(bs[0][:, 0:qw], lhsT=KT[:, kt2 * 128:(kt2 + 1) * 128], rhs=QT[:, q0:q0 + qw], start=True, stop=True),
                                  R=[kK, kQ], W=[bs[1]])
                            fw.op('act', lambda e: e.activation(PT[pb2][:, 0:qw], bs[0][:, 0:qw], AF.Exp, scale=scale), R=[bs[1]], W=['da_pt%d' % pb2])
                        score(0)
                        for ki, kt_ in enumerate(ktiles):
                            pb = (npt + ki) % 2
                            if ki + 1 < len(ktiles):
                                score(ki + 1)
                            for qt_ in range(nqt):
                                bo = self.banks[4 + qt_]
                                fw.op('pe', lambda e: e.matmul(bo[0][:, 0:257], lhsT=PT[pb][:, qt_ * 128:(qt_ + 1) * 128], rhs=va[:, kt_, :],
                                                               start=(ki == 0), stop=(ki == len(ktiles) - 1)), R=['da_pt%d' % pb, 'da_va'], W=[bo[1]])
                        npt += len(ktiles)
                        for qt_ in range(nqt):
                            bo = self.banks[4 + qt_]
                            i = q0 // 128 + qt_
                            fw.op('dve', lambda e: e.reciprocal(rs[:], bo[0][:, 256:257]), R=[bo[1]], W=['da_rs1'])
                            if a == 0:
                                fw.op('dve', lambda e: e.tensor_scalar(o1[:, qt_, :], bo[0][:, 0:256], rs[:, 0:1], None, ALU.mult), R=[bo[1], 'da_rs1'], W=['da_o1'])
                            else:
                                fw.op('dve', lambda e: e.tensor_scalar(o2[:], bo[0][:, 0:256], rs[:, 0:1], None, ALU.mult), R=[bo[1], 'da_rs1'], W=['da_o2'])
                                fw.op('dve', lambda e: e.scalar_tensor_tensor(o2[:], o2[:], nlam[:, 0:1], o1[:, qt_, :], ALU.mult, ALU.add),
                                      R=['da_o2', 'da_o1', 'da_nlam'], W=['da_o2'])
                                fw.op('act', lambda e: e.activation(junk[:], o2[:], AF.Square, accum_out=ss[:]), R=['da_o2'], W=['da_ss', 'da_junk'])
                                fw.op('act', lambda e: e.activation(ss[:], ss[:], AF.Sqrt, bias=self.epsc[:], scale=1.0 / 256), R=['da_ss', 'eps'], W=['da_ss'])
                                fw.op('dve', lambda e: e.reciprocal(ss[:], ss[:]), R=['da_ss'], W=['da_ss'])
                                fw.op('dve', lambda e: e.scalar_tensor_tensor(og[:], o2[:], ss[:, 0:1], ngb[:], ALU.mult, ALU.mult), R=['da_o2', 'da_ss', 'da_ngb'], W=['da_og'])
                                for c in range(2):
                                    bt = self.banks[2 + c]
                                    pt = bt[0].bitcast(BF16)
                                    fw.op('pe', lambda e: e.transpose(pt[:, 0:128], og[:, c * 128:(c + 1) * 128], self.identb[:]), R=['da_og', 'identb'], W=[bt[1]])
                                    fw.op('act', lambda e: e.copy(ogT[c][:, i * 128:(i + 1) * 128], pt[:, 0:128]), R=[bt[1]], W=['da_ogT%d' % c])
                for c in range(2):
                    fw.dma('sp', self.OGT[2 * h + c], ogT[c][:], R=['da_ogT%d' % c], W=['OGT'])
            fw.barrier()

    def mlstm(self, layer):
        fw = self.fw
        j = layer // 3
        W_in = self.I["mlstm_w_in"][j]
        CAP = 15.0
        with ExitStack() as es:
            G = self.sb(es, "ml_G", [32, TT], F32); LF = self.sb(es, "ml_LF", [32, TT], F32)
            gb = self.sb(es, "ml_gb", [32, 1], F32)
            gsel = self.sb(es, "ml_gsel", [32, 32 * 128], F32)
            kTf = self.sb(es, "ml_kTf", [128, TT], BF16); qTf = self.sb(es, "ml_qTf", [128, TT], BF16)
            self.rmask = self.sb(es, "ml_rmask", [128, TT], BF16)
            va = self.sb(es, "ml_va", [128, NT, 257], BF16)
            sgo = self.sb(es, "ml_sgo", [128, NT, 256], BF16)
            hacc = self.sb(es, "ml_hacc", [128, NT, 256], F32)
            ogT = [self.sb(es, "ml_ogT%d" % i, [128, TT], BF16) for i in range(2)]
            ngb = self.sb(es, "ml_ngb", [128, 256], F32)
            ss = self.sb(es, "ml_ss", [128, NT], F32); junk = self.sb(es, "ml_junk", [128, 256], F32)
            dn = self.sb(es, "ml_dn", [128, 1], F32)
            og = [self.sb(es, "ml_og%d" % i, [128, 256], BF16) for i in range(2)]
            fw.op('pool', lambda e: e.memset(self.rmask[:], 1.0), W=['rmask'])
            fw.op('pool', lambda e: e.memset(self.rmask[:].rearrange("p (n l) -> p n l", l=128)[:, :, 0:1], 0.0), W=['rmask'])
            fw.op('dve', lambda e: e.memset(va[:, :, 256:257], 1.0), W=['ml_va'])
            fw.dma('sp', gsel[:], self.I["gsel"], W=['ml_gsel'])
            fw.dma('sp', gb[:], self.I["mlstm_gb"], W=['ml_gb'])
            fw.op('dve', lambda e: e.tensor_scalar(gb[:], gb[:], 1.0 / CAP, None, ALU.mult), R=['ml_gb'], W=['ml_gb'])
            blocks = [(0, 512), (512, 512), (1024, 512), (1536, 512), (2048, 256)]
            with ExitStack() as es1:
                wgt = self.sb(es1, "ml_wgt", [128, KC, 32], BF16)
                hb = self.sb(es1, "ml_hb0", [128, KC, 512], BF16)
                fw.dma('pool', wgt[:], W_in[:, 3072:3104].rearrange("(k p) c -> p k c", p=128), W=['ml_wgt'])
                for (t0, bw) in blocks:
                    fw.dma('sp', hb[:, :, 0:bw], self.HT.rearrange("k p t -> p k t")[:, :, t0:t0 + bw], R=['HT'], W=['ml_hb0'])
                    bk = self.banks[0]
                    for k in range(KC):
                        fw.op('pe', lambda e, k=k: e.matmul(bk[0][0:32, 0:bw], lhsT=wgt[:, k, :], rhs=hb[:, k, 0:bw], start=(k == 0), stop=(k == KC - 1)),
                              R=['ml_wgt', 'ml_hb0'], W=[bk[1]])
                    fw.op('act', lambda e: e.activation(G[:, t0:t0 + bw], bk[0][0:32, 0:bw], AF.Tanh, bias=gb[:], scale=1.0 / CAP), R=[bk[1], 'ml_gb'], W=['ml_G'])
                fw.op('dve', lambda e: e.tensor_scalar(G[:], G[:], CAP, None, ALU.mult), R=['ml_G'], W=['ml_G'])
                fw.op('act', lambda e: e.activation(LF[:], G[:], AF.Exp, scale=-1.0), R=['ml_G'], W=['ml_LF'])
                fw.op('act', lambda e: e.activation(LF[:], LF[:], AF.Ln, bias=self.onec[0:32, :]), R=['ml_LF'], W=['ml_LF'])
                fw.op('dve', lambda e: e.tensor_scalar(LF[:], LF[:], -1.0, None, ALU.mult), R=['ml_LF'], W=['ml_LF'])
                fw.barrier()
            for h in range(8):
                with ExitStack() as es1:
                    wp = self.sb(es1, "ml_w%d" % h, [128, KC, 768], BF16)
                    hb = self.sb(es1, "ml_hb%d" % (h + 1), [128, KC, 512], BF16)
                    fw.dma('pool', wp[:, :, 0:128], W_in[:, h * 128:(h + 1) * 128].rearrange("(k p) c -> p k c", p=128), W=['ml_w'])
                    fw.dma('pool', wp[:, :, 128:256], W_in[:, 3104 + h * 128:3104 + (h + 1) * 128].rearrange("(k p) c -> p k c", p=128), W=['ml_w'])
                    fw.dma('pool', wp[:, :, 256:512], W_in[:, 1024 + h * 256:1024 + (h + 1) * 256].rearrange("(k p) c -> p k c", p=128), W=['ml_w'])
                    fw.dma('pool', wp[:, :, 512:768], W_in[:, 4128 + h * 256:4128 + (h + 1) * 256].rearrange("(k p) c -> p k c", p=128), W=['ml_w'])
                    fw.dma('sp', ngb[:], self.I["mlstm_ng"][0:1, h * 256:(h + 1) * 256].partition_broadcast(128)[:, 0, :], W=['ml_ngb'])
                    for (t0, bw) in blocks:
                        fw.dma('sp', hb[:, :, 0:bw], self.HT.rearrange("k p t -> p k t")[:, :, t0:t0 + bw], R=['HT'], W=['ml_hb'])
                        for g in range(2):
                            bk = self.banks[g]
                            for k in range(KC):
                                fw.op('pe', lambda e, k=k: e.matmul(bk[0][:, 0:bw], lhsT=wp[:, k, g * 128:(g + 1) * 128], rhs=hb[:, k, 0:bw],
                                                                    start=(k == 0), stop=(k == KC - 1)), R=['ml_w', 'ml_hb'], W=[bk[1]])
                            if g == 0:
                                fw.op('act', lambda e: e.copy(kTf[:, t0:t0 + bw], bk[0][:, 0:bw]), R=[bk[1]], W=['ml_kTf'])
                            else:
                                fw.op('act', lambda e: e.activation(qTf[:, t0:t0 + bw], bk[0][:, 0:bw], AF.Copy, scale=128 ** -0.5), R=[bk[1]], W=['ml_qTf'])
                        for tl in range(bw // 128):
                            i = t0 // 128 + tl
                            for g in range(2):
                                bk = self.banks[2 + g]
                                for k in range(KC):
                                    fw.op('pe', lambda e, k=k: e.matmul(bk[0][:, 0:256], lhsT=hb[:, k, tl * 128:(tl + 1) * 128], rhs=wp[:, k, 256 + g * 256:512 + g * 256],
                                                                        start=(k == 0), stop=(k == KC - 1)), R=['ml_w', 'ml_hb'], W=[bk[1]])
                                if g == 0:
                                    fw.op('dve', lambda e: e.tensor_copy(va[:, i, 0:256], bk[0][:, 0:256]), R=[bk[1]], W=['ml_va'])
                                else:
                                    fw.op('act', lambda e: e.activation(sgo[:, i, :], bk[0][:, 0:256], AF.Sigmoid), R=[bk[1]], W=['ml_sgo'])
                    fw.op('pool', lambda e: e.tensor_tensor(sgo[:], sgo[:], ngb[:].rearrange("p (o c) -> p o c", o=1).to_broadcast([128, NT, 256]), ALU.mult),
                          R=['ml_sgo', 'ml_ngb'], W=['ml_sgo'])
                    fw.barrier()
                for d in range(2):
                    with ExitStack() as es2:
                        IW = self.sb(es2, "ml_IW%d_%d" % (h, d), [128, TT], F32); LFb = self.sb(es2, "ml_LFb%d_%d" % (h, d), [128, TT], F32)
                        A = self.sb(es2, "ml_A%d_%d" % (h, d), [128, TT], F32); TMP = self.sb(es2, "ml_T%d_%d" % (h, d), [128, TT], F32)
                        self.dp_e1 = self.sb(es2, "ml_e1%d_%d" % (h, d), [128, TT], F32)
                        qt = self.sb(es2, "ml_qt%d_%d" % (h, d), [128, TT], BF16); kt = self.sb(es2, "ml_kt%d_%d" % (h, d), [128, TT], BF16)
                        khT = self.sb(es2, "ml_khT%d_%d" % (h, d), [128, TT], BF16); kh = self.sb(es2, "ml_kh%d_%d" % (h, d), [128, NT, 128], BF16)
                        emid = self.sb(es2, "ml_em%d_%d" % (h, d), [128, NT], F32); eend = self.sb(es2, "ml_ee%d_%d" % (h, d), [128, NT], F32)
                        ri = (2 * d) * 8 + h
                        rf = (2 * d + 1) * 8 + h
                        for (src, ksrc, r, dst, kdst) in ((G, 'ml_G', ri, IW, 'ml_IW'), (LF, 'ml_LF', rf, LFb, 'ml_LFb')):
                            for bi, (t0, bw) in enumerate(blocks):
                                bk = self.banks[bi % 2]
                                fw.op('pe', lambda e: e.matmul(bk[0][:, 0:bw], lhsT=gsel[:, r * 128:(r + 1) * 128], rhs=src[:, t0:t0 + bw], start=True, stop=True),
                                      R=['ml_gsel', ksrc], W=[bk[1]])
                                fw.op('act', lambda e: e.copy(dst[:, t0:t0 + bw], bk[0][:, 0:bw]), R=[bk[1]], W=[kdst])
                        pfx = 'ml'
                        self.decay_prep(pfx, d, LFb, A, TMP, qTf, kTf, qt, kt, khT, emid, eend,
                                        {'lf': 'ml_LFb', 'q': 'ml_qTf', 'k': 'ml_kTf', 'iw': 'ml_IW'}, iw=IW)
                        self.to_tokmajor(khT, pfx + 'khT', kh, pfx + 'kh', self.banks[0])

                        def post(i, ops, kops, d=d):
                            fw.op('dve', lambda e: e.tensor_scalar(dn[:], ops[:, 256:257], -1.0, None, ALU.mult), R=[kops], W=['ml_dn'])
                            fw.op('dve', lambda e: e.scalar_tensor_tensor(dn[:], ops[:, 256:257], 1.0, dn[:], ALU.max, ALU.max), R=[kops, 'ml_dn'], W=['ml_dn'])
                            fw.op('dve', lambda e: e.reciprocal(dn[:], dn[:]), R=['ml_dn'], W=['ml_dn'])
                            if d == 0:
                                fw.op('dve', lambda e: e.tensor_scalar(hacc[:, i, :], ops[:, 0:256], dn[:, 0:1], None, ALU.mult), R=[kops, 'ml_dn'], W=['ml_hacc'])
                            else:
                                fw.op('dve', lambda e: e.scalar_tensor_tensor(hacc[:, i, :], ops[:, 0:256], dn[:, 0:1], hacc[:, i, :], ALU.mult, ALU.add),
                                      R=[kops, 'ml_dn', 'ml_hacc'], W=['ml_hacc'])
                        self.gla(es2, pfx + 'g', 257, qt, kt, kh, va, emid, eend,
                                 {'q': pfx + 'qt', 'k': pfx + 'kt', 'kh': pfx + 'kh', 'v': 'ml_va', 'e': pfx + 'e'}, d, post)
                        fw.barrier()
                for i in range(NT):
                    fw.op('act', lambda e, i=i: e.activation(junk[:], hacc[:, i, :], AF.Square, accum_out=ss[:, i:i + 1]), R=['ml_hacc'], W=['ml_ss', 'ml_junk'])
                fw.op('act', lambda e: e.activation(ss[:], ss[:], AF.Sqrt, bias=self.epsc[:], scale=1.0 / 256), R=['ml_ss', 'eps'], W=['ml_ss'])
                fw.op('dve', lambda e: e.reciprocal(ss[:], ss[:]), R=['ml_ss'], W=['ml_ss'])
                for i in range(NT):
                    b = i % 2
                    fw.op('dve', lambda e, i=i, b=b: e.scalar_tensor_tensor(og[b][:], hacc[:, i, :], ss[:, i:i + 1], sgo[:, i, :], ALU.mult, ALU.mult),
                          R=['ml_hacc', 'ml_ss', 'ml_sgo'], W=['ml_og%d' % b])
                    for c in range(2):
                        bt = self.banks[b * 2 + c]
                        pt = bt[0].bitcast(BF16)
                        fw.op('pe', lambda e: e.transpose(pt[:, 0:128], og[b][:, c * 128:(c + 1) * 128], self.identb[:]), R=['ml_og%d' % b, 'identb'], W=[bt[1]])
                        fw.op('act', lambda e: e.copy(ogT[c][:, i * 128:(i + 1) * 128], pt[:, 0:128]), R=[bt[1]], W=['ml_ogT%d' % c])
                for c in range(2):
                    fw.dma('sp', self.OGT[2 * h + c], ogT[c][:], R=['ml_ogT%d' % c], W=['OGT'])
            fw.barrier()

    def phaseC(self, layer, W_out=None):
        fw = self.fw
        kind, j = layer % 3, layer // 3
        W_out = self.I[["hgrn_w_out", "diff_w_out", "mlstm_w_out"][kind]][j]
        full_ctx = layer < DEPTH - 1
        with ExitStack() as es:
            og = self.sb(es, "pc_og", [128, KC, TT], BF16)
            wb = [self.sb(es, "pc_w%d" % i, [128, KC, 512], BF16) for i in range(2)]
            g1 = self.sb(es, "pc_g1", [128, D], F32); g1c = self.sb(es, "pc_g1c", [128, D], F32)
            xt = [self.sb(es, "pc_x%d" % i, [128, 512], F32) for i in range(3)]
            for k in range(KC):
                fw.dma('sp', og[:, k, :], self.OGT[k], R=['OGT'], W=['pc_og'])
            fw.dma('sp', g1[:], self.MOD[0:1, 2 * D:3 * D].partition_broadcast(128)[:, 0, :], R=['MOD'], W=['pc_g1'])
            fw.dma('sp', g1c[:], self.MOD[1:2, 2 * D:3 * D].partition_broadcast(128)[:, 0, :], R=['MOD'], W=['pc_g1c'])
            n = 0
            for nb in range(4):
                w = wb[nb % 2]; kw = 'pc_w%d' % (nb % 2)
                fw.dma('pool', w[:], W_out[:, nb * 512:(nb + 1) * 512].rearrange("(k p) f -> p k f", p=128), W=[kw])
                for i in range(NT):
                    if i < 2 and not full_ctx:
                        continue
                    b = n % 3; n += 1
                    x_, kx = xt[b], 'pc_x%d' % b
                    src, ksrc = self.src_rows(layer, i)
                    fw.dma('sp', x_[:], src[:, nb * 512:(nb + 1) * 512], R=[ksrc], W=[kx])
                    bk = self.banks[n % 8]
                    for k in range(KC):
                        fw.op('pe', lambda e, k=k: e.matmul(bk[0], lhsT=og[:, k, i * 128:(i + 1) * 128], rhs=w[:, k, :],
                                                            start=(k == 0), stop=(k == KC - 1)), R=['pc_og', kw], W=[bk[1]])
                    gg, kg = (g1, 'pc_g1') if i >= 2 else (g1c, 'pc_g1c')
                    fw.op('dve', lambda e: e.tensor_tensor(bk[0], bk[0], gg[:, nb * 512:(nb + 1) * 512], ALU.mult), R=[bk[1], kg], W=[bk[1]])
                    fw.op('dve', lambda e: e.tensor_tensor(x_[:], x_[:], bk[0], ALU.add), R=[bk[1], kx], W=[kx])
                    rows = slice(i * 128, (i + 1) * 128)
                    cols = slice(nb * 512, (nb + 1) * 512)
                    fw.dma('sp', self.XA[rows, cols], x_[:], R=[kx], W=['XA'])
                    fw.dma('sp', self.XB[rows, cols], x_[:], R=[kx], W=['XB%d' % i])
            fw.barrier()

    def moe(self, layer):
        fw = self.fw
        nc = self.nc
        full_ctx = layer < DEPTH - 1
        ntile = NT if full_ctx else NT
        with ExitStack() as eso:
            idxTi = self.sb(eso, "mo_idxTi", [128, 3, NE], I32)
            gT = self.sb(eso, "mo_gT", [128, 3, NE], F32)
            with ExitStack() as es:
                xt = [self.sb(es, "mr_x%d" % i, [128, D], F32) for i in range(2)]
                h32 = [self.sb(es, "mr_h%d" % i, [128, KC, 128], F32) for i in range(2)]
                ss = self.sb(es, "mr_ss", [128, 1], F32); sd = self.sb(es, "mr_sd", [128, 1], F32)
                junk = self.sb(es, "mr_junk", [128, D], BF16)
                wr = self.sb(es, "mr_wr", [128, KC, NE], F32)
                affT = self.sb(es, "mr_affT", [NE, TT], F32)
                ex = self.sb(es, "mr_ex", [128, NE], F32); mx = self.sb(es, "mr_mx", [128, 1], F32); sm = self.sb(es, "mr_sm", [128, 1], F32)
                vals = self.sb(es, "mr_vals", [NE, 288], F32); idx = self.sb(es, "mr_idx", [NE, 288], U32)
                idxf = self.sb(es, "mr_idxf", [NE, 288], F32)
                tT = self.sb(es, "mr_tT", [128, 2, NE], F32)
                fw.dma('sp', wr[:], self.I["moe_router"][layer].rearrange("(k p) e -> p k e", p=128), W=['mr_wr'])
                for i in range(NT):
                    if i < 2 and not full_ctx:
                        continue
                    b = i % 2; kx = 'mr_x%d' % b; kh = 'mr_h%d' % b
                    fw.dma('sp', xt[b][:], self.XA[i * 128:(i + 1) * 128, :], W=[kx])
                    self.norm_tile(xt[b], kx, 128, 4 if i >= 2 else 6, h32[b], kh, (ss, sd, junk), 'mr_t',
                                   bank_ids=(0, 1, 2, 3))
                    pl = self.banks[4 + b]
                    for k in range(KC):
                        fw.op('pe', lambda e, k=k: e.matmul(pl[0][:, 0:NE], lhsT=h32[b][:, k, :], rhs=wr[:, k, :], start=(k == 0), stop=(k == KC - 1)),
                              R=[kh, 'mr_wr'], W=[pl[1]])
                    fw.op('dve', lambda e: e.tensor_reduce(mx[:], pl[0][:, 0:NE], AX.X, ALU.max, negate=True), R=[pl[1]], W=['mr_mx'])
                    fw.op('act', lambda e: e.activation(ex[:], pl[0][:, 0:NE], AF.Exp, bias=mx[:], accum_out=sm[:]), R=[pl[1], 'mr_mx'], W=['mr_ex', 'mr_sm'])
                    fw.op('dve', lambda e: e.reciprocal(sm[:], sm[:]), R=['mr_sm'], W=['mr_sm'])
                    fw.op('dve', lambda e: e.tensor_scalar(ex[:], ex[:], sm[:, 0:1], None, ALU.mult), R=['mr_ex', 'mr_sm'], W=['mr_ex'])
                    pt = self.banks[6 + b]
                    fw.op('pe', lambda e: e.transpose(pt[0][0:NE, 0:128], ex[:], self.ident[:]), R=['mr_ex', 'ident'], W=[pt[1]])
                    fw.op('dve', lambda e: e.tensor_copy(affT[:, i * 128:(i + 1) * 128], pt[0][0:NE, 0:128]), R=[pt[1]], W=['mr_affT'])
                def topk(work, nround, o0):
                    for r in range(nround):
                        sl = slice(o0 + r * 8, o0 + (r + 1) * 8)
                        fw.op('dve', lambda e: e.max(vals[:, sl], work), R=['mr_affT'], W=['mr_vals'])
                        fw.op('dve', lambda e: e.max_index(idx[:, sl], vals[:, sl], work), R=['mr_affT', 'mr_vals'], W=['mr_idx'])
                        fw.op('dve', lambda e: e.match_replace(work, vals[:, sl], work, -1.0), R=['mr_vals', 'mr_idx'], W=['mr_affT'])
                topk(affT[:, 256:TT], 32, 0)
                if full_ctx:
                    topk(affT[:, 0:256], 4, 256)
                else:
                    fw.op('dve', lambda e: e.memset(vals[:, 256:288], 0.0), W=['mr_vals'])
                    fw.op('dve', lambda e: e.memset(idx[:, 256:288], 0), W=['mr_idx'])
                fw.op('dve', lambda e: e.tensor_copy(idxf[:], idx[:]), R=['mr_idx'], W=['mr_idxf'])
                fw.op('dve', lambda e: e.tensor_scalar(idxf[:, 0:256], idxf[:, 0:256], 256.0, None, ALU.add), R=['mr_idxf'], W=['mr_idxf'])
                for (src, ksrc, dst, kdst) in ((idxf, 'mr_idxf', idxTi, 'mo_idxTi'), (vals, 'mr_vals', gT, 'mo_gT')):
                    pt = self.banks[0]
                    for st in range(2):
                        fw.op('pe', lambda e, st=st: e.transpose(pt[0][:, st * NE:(st + 1) * NE], src[:, st * 128:(st + 1) * 128], self.ident[0:NE, 0:NE]),
                              R=[ksrc, 'ident'], W=[pt[1]])
                    fw.op('pe', lambda e: e.transpose(pt[0][0:32, 2 * NE:3 * NE], src[:, 256:288], self.ident[0:NE, 0:NE]), R=[ksrc, 'ident'], W=[pt[1]])
                    fw.op('dve', lambda e: e.tensor_copy(dst[:, 0:2, :].rearrange("p a b -> p (a b)"), pt[0][:, 0:2 * NE]), R=[pt[1]], W=[kdst])
                    fw.op('dve', lambda e: e.tensor_copy(dst[0:32, 2, :], pt[0][0:32, 2 * NE:3 * NE]), R=[pt[1]], W=[kdst])
                fw.barrier()
            with ExitStack() as es:
                nst = 3 if full_ctx else 2
                NS = 288 if full_ctx else 256
                xg = [self.sb(es, "me_xg%d" % i, [128, D], F32) for i in range(3)]
                junk = self.sb(es, "me_junk", [128, D], BF16)
                ss = self.sb(es, "me_ss", [128, 1], F32); sd = self.sb(es, "me_sd", [128, 1], F32)
                xsT = self.sb(es, "me_xsT", [128, KC, 288], BF16)
                wg = [self.sb(es, "me_wg%d" % i, [128, KC, 256], BF16) for i in range(4)]
                wu = [self.sb(es, "me_wu%d" % i, [128, KC, 256], BF16) for i in range(4)]
                wd = self.sb(es, "me_wd", [128, NF, D], BF16)
                hmT = self.sb(es, "me_hmT", [128, NF, 288], BF16)
                sg = [self.sb(es, "me_sg%d" % i, [128, 288], F32) for i in range(2)]
                yo = [self.sb(es, "me_yo%d" % i, [128, D], F32) for i in range(2)]
                g2b = self.sb(es, "me_g2b", [128, D], F32); g2cb = self.sb(es, "me_g2cb", [128, D], F32)
                fw.dma('sp', g2b[:], self.MOD[0:1, 5 * D:6 * D].partition_broadcast(128)[:, 0, :], W=['me_g2b'])
                fw.dma('sp', g2cb[:], self.MOD[1:2, 5 * D:6 * D].partition_broadcast(128)[:, 0, :], W=['me_g2cb'])
                Wg, Wu, Wd = self.I["moe_w_gate"][layer], self.I["moe_w_up"][layer], self.I["moe_w_down"][layer]
                nyo = 0
                xsTs = [xsT, self.sb(es, "me_xsT2", [128, KC, 288], BF16)]

                def gather(ex_):
                    for st in range(nst):
                        n = 128 if st < 2 else 32
                        fw.dma('pool', None, None, R=['mo_idxTi', 'XA'], W=['me_xg%d' % st],
                               fn=lambda g, st=st, n=n: g.indirect_dma_start(
                                   out=xg[st][0:n, :], out_offset=None, in_=self.XA[:, :],
                                   in_offset=bass.IndirectOffsetOnAxis(ap=idxTi[0:n, st, ex_:ex_ + 1], axis=0)))

                def norm1(ex_, st):
                    xs_ = xsTs[ex_ % 2]
                    n = 128 if st < 2 else 32
                    self.norm_tile(xg[st], 'me_xg%d' % st, n, 4 if st < 2 else 6, xs_[:, :, st * 128:st * 128 + n], 'me_xsT%d' % (ex_ % 2),
                                   (ss, sd, junk), 'me_t', bank_ids=(4, 5, 6, 7))

                pieces = [(e_, pc) for e_ in range(NE) for pc in range(6)]
                issued = [0]

                def issue_upto(m):
                    while issued[0] < min(m, len(pieces)):
                        e_, pc = pieces[issued[0]]
                        b = issued[0] % 4
                        f0 = pc * 256
                        fwd_ = min(256, FF - f0)
                        fw.dma('pool', wg[b][:, :, 0:fwd_], Wg[e_][:, f0:f0 + fwd_].rearrange("(k p) f -> p k f", p=128), W=['me_wg%d' % b])
                        fw.dma('pool', wu[b][:, :, 0:fwd_], Wu[e_][:, f0:f0 + fwd_].rearrange("(k p) f -> p k f", p=128), W=['me_wu%d' % b])
                        issued[0] += 1
                gather(0)
                for st in range(nst):
                    norm1(0, st)
                issue_upto(3)
                for ex_ in range(NE):
                    xs_ = xsTs[ex_ % 2]; kxs = 'me_xsT%d' % (ex_ % 2)
                    if ex_ + 1 < NE:
                        gather(ex_ + 1)
                    ft = 0
                    for pc in range(6):
                        pi = ex_ * 6 + pc
                        issue_upto(pi + 4)
                        if pc == 1:
                            fw.dma('pool', wd[:], Wd[ex_].rearrange("(c p) d -> p c d", p=128), W=['me_wd'])
                        f0 = pc * 256
                        fwd_ = min(256, FF - f0)
                        b = pi % 4
                        for fl in range(fwd_ // 128):
                            pb = ft % 2
                            pg, pu = self.banks[pb * 2], self.banks[pb * 2 + 1]
                            for (w_, kw_, p_) in ((wg[b], 'me_wg%d' % b, pg), (wu[b], 'me_wu%d' % b, pu)):
                                for k in range(KC):
                                    fw.op('pe', lambda e, k=k: e.matmul(p_[0][:, 0:NS], lhsT=w_[:, k, fl * 128:(fl + 1) * 128], rhs=xs_[:, k, 0:NS],
                                                                        start=(k == 0), stop=(k == KC - 1)), R=[kw_, kxs], W=[p_[1]])
                            fw.op('act', lambda e: e.activation(sg[pb][:, 0:NS], pg[0][:, 0:NS], AF.Silu), R=[pg[1]], W=['me_sg%d' % pb])
                            fw.op('dve', lambda e: e.tensor_tensor(hmT[:, ft, 0:NS], sg[pb][:, 0:NS], pu[0][:, 0:NS], ALU.mult),
                                  R=['me_sg%d' % pb, pu[1]], W=['me_hmT'])
                            ft += 1
                        if ex_ + 1 < NE and 2 <= pc < 2 + nst:
                            norm1(ex_ + 1, pc - 2)
                    for st in range(nst):
                        n = 128 if st < 2 else 32
                        for nb in range(4):
                            bk = self.banks[4 + nb]
                            for c in range(NF):
                                fw.op('pe', lambda e, c=c: e.matmul(bk[0][0:n, :], lhsT=hmT[:, c, st * 128:st * 128 + n], rhs=wd[:, c, nb * 512:(nb + 1) * 512],
                                                                    start=(c == 0), stop=(c == NF - 1)), R=['me_hmT', 'me_wd'], W=[bk[1]])
                        yb = nyo % 2; nyo += 1
                        gg, kg = (g2b, 'me_g2b') if st < 2 else (g2cb, 'me_g2cb')
                        fw.op('dve', lambda e: e.scalar_tensor_tensor(yo[yb][0:n, :], self.P1[0:n, :], gT[0:n, st, ex_:ex_ + 1], gg[0:n, :], ALU.mult, ALU.mult),
                              R=['pb4', 'pb5', 'pb6', 'pb7', 'mo_gT', kg], W=['me_yo%d' % yb])
                        fw.dma('pool', None, None, R=['me_yo%d' % yb, 'mo_idxTi'], W=['XBs'],
                               fn=lambda g, st=st, n=n, yb=yb: g.indirect_dma_start(
                                   out=self.XB[:, :], out_offset=bass.IndirectOffsetOnAxis(ap=idxTi[0:n, st, ex_:ex_ + 1], axis=0),
                                   in_=yo[yb][0:n, :], in_offset=None, compute_op=ALU.add))
                fw.barrier()

    def final_norm(self):
        fw = self.fw
        with ExitStack() as es:
            xt = [self.sb(es, "fn_x%d" % i, [128, D], F32) for i in range(2)]
            fg = self.sb(es, "fn_g", [128, D], F32)
            ss = self.sb(es, "fn_ss", [128, 1], F32); junk = self.sb(es, "fn_junk", [128, D], BF16)
            epsc = self.sb(es, "fn_eps", [128, 1], F32)
            fw.op('dve', lambda e: e.memset(epsc[:], EPS), W=['fn_eps'])
            fw.dma('sp', fg[:], self.I["final_g"].partition_broadcast(128)[:, 0, :], W=['fn_g'])
            for i in range(2, NT):
                b = i % 2; kx = 'fn_x%d' % b
                fw.dma('sp', xt[b][:], self.XB[i * 128:(i + 1) * 128, :], R=['XB'], W=[kx])
                fw.op('act', lambda e: e.activation(junk[:], xt[b][:], AF.Square, accum_out=ss[:]), R=[kx], W=['fn_ss'])
                fw.op('act', lambda e: e.activation(ss[:], ss[:], AF.Sqrt, bias=epsc[:], scale=1.0 / D), R=['fn_ss', 'fn_eps'], W=['fn_ss'])
                fw.op('dve', lambda e: e.reciprocal(ss[:], ss[:]), R=['fn_ss'], W=['fn_ss'])
                fw.op('dve', lambda e: e.scalar_tensor_tensor(xt[b][:], xt[b][:], ss[:, 0:1], fg[:], ALU.mult, ALU.mult), R=[kx, 'fn_ss', 'fn_g'], W=[kx])
                fw.dma('sp', self.out[(i - 2) * 128:(i - 1) * 128, :], xt[b][:], R=[kx])


def make_consts():
    c = {}
    c["ident"] = np.eye(128, dtype=np.float32)
    s = np.arange(128)[:, None]; t = np.arange(128)[None, :]
    c["mask_f"] = (t >= s).astype(np.uint8)
    c["mask_b"] = (t <= s).astype(np.uint8)
    T = 2048; GW = 64
    pos = np.arange(T)
    row = (pos // GW).astype(np.float32); col = (pos % GW).astype(np.float32)
    half = 64
    inv = (1.0 / (np.float32(10000.0) ** (np.arange(0, half, 2, dtype=np.float32) / np.float32(half)))).astype(np.float32)
    ar = row[None, :] * inv[:, None]
    ac = col[None, :] * inv[:, None]
    C = np.concatenate([np.cos(ar), np.cos(ar), np.cos(ac), np.cos(ac)], axis=0)
    S = np.concatenate([-np.sin(ar), np.sin(ar), -np.sin(ac), np.sin(ac)], axis=0)
    c["rope_c"] = C.astype(np.float32); c["rope_s"] = S.astype(np.float32)
    perm = np.zeros((128, 128), np.float32)
    for m in range(128):
        blk = m // 32
        sw = (blk ^ 1) * 32 + (m % 32)
        perm[sw, m] = 1.0
    c["perm"] = perm
    gsel = np.zeros((32, 32, 128), np.float32)
    for r in range(32):
        gsel[r, r, :] = 1.0
    c["gsel"] = gsel.reshape(32, 32 * 128)
    return c

def prep_shared(inp):
    d = {}
    for k in ["ada_w", "ada_b", "hgrn_w_in", "hgrn_w_out", "diff_w_in", "diff_w_out", "mlstm_w_in", "mlstm_w_out",
              "moe_router", "moe_w_gate", "moe_w_up", "moe_w_down"]:
        d[k] = np.ascontiguousarray(inp[k])
    d["norm_g"] = np.ascontiguousarray(inp["norm_g"]).reshape(8, 2048)
    d["final_g"] = np.ascontiguousarray(inp["final_g"]).reshape(1, 2048)
    d["hgrn_lb"] = np.ascontiguousarray(inp["hgrn_lb_logits"]).reshape(4, 2048)
    d["hgrn_ng"] = np.ascontiguousarray(inp["hgrn_norm_g"]).reshape(2, 128)
    d["diff_lam"] = np.ascontiguousarray(inp["diff_lambda"]).reshape(4, 128)
    d["diff_ng"] = np.ascontiguousarray(inp["diff_norm_g"]).reshape(1, 256)
    d["mlstm_gb"] = np.ascontiguousarray(inp["mlstm_gate_b"]).reshape(32, 1)
    d["mlstm_ng"] = np.ascontiguousarray(inp["mlstm_norm_g"]).reshape(1, 2048)
    d.update(make_consts())
    return d

def prep_core(inp, b):
    return {"x": np.ascontiguousarray(inp["x"][b]), "ctx": np.ascontiguousarray(inp["ctx"][b]),
            "cc": np.ascontiguousarray(np.stack([inp["c"][b], inp["c_ctx"]], axis=0))}


_CACHE = {}


def kernel(**inputs):
    inputs = {k: np.asarray(v) for k, v in inputs.items()}
    if "nc" not in _CACHE:
        bld = Builder(nlayers=DEPTH, dbg=False)
        _CACHE["nc"] = bld.build()
        _CACHE["used"] = set(bld.I.keys())
    nc = _CACHE["nc"]
    used = _CACHE["used"]
    shared = {k: v for k, v in prep_shared(inputs).items() if k in used}
    n = 8
    in_maps = []
    for b in range(n):
        m = dict(shared)
        m.update(prep_core(inputs, b))
        in_maps.append(m)
    res = run_bass_kernel_spmd(nc, in_maps, core_ids=list(range(n)))
    out = np.stack([np.asarray(res.results[b]["out"]) for b in range(n)], axis=0)
    return out.astype(np.float32, copy=False)
```
